# Optimizing a Trainium2 kernel written in Bass

```python
import jax, jax.numpy as jnp
from jax import lax
import numpy as np

D_MODEL = 1024
BATCH = 16
SEQ = 2048
DEPTH = 2

HEAD_DIM = 64
GROUP_HEADS = 4
GROUP_WIDTH = GROUP_HEADS * HEAD_DIM
N_GROUPS = 4
D_MIX = N_GROUPS * GROUP_WIDTH
SB_COLS = 3 * GROUP_WIDTH
CONF_COLS = 2 * GROUP_WIDTH
GMLP_COLS = 2 * GROUP_WIDTH
LRU_COLS = 2 * GROUP_WIDTH
IN_COLS = SB_COLS + CONF_COLS + GMLP_COLS + LRU_COLS
SB_BLOCK = 128
CONF_KERNEL = 31
GMLP_CHUNK = 128
LRU_CONV = 4
LRU_C = 8.0
D_FF = 2816
N_EXPERTS = 8
TOP_K = 2
D_FF_EXPERT = 3584
EXPERT_BLOCK = 512
N_DENSE = (DEPTH + 1) // 2
N_MOE = DEPTH // 2
EPS = 1e-6

kernel_name = "hybrid_sb_conformer_gmlp_rglru_moe"


def rms_norm(x, g):
    xf = x.astype(jnp.float32)
    y = xf * lax.rsqrt(jnp.mean(xf * xf, axis=-1, keepdims=True) + EPS)
    return (y * g.astype(jnp.float32)).astype(x.dtype)


def layer_norm(x, g, b):
    xf = x.astype(jnp.float32)
    xc = xf - jnp.mean(xf, axis=-1, keepdims=True)
    var = jnp.mean(xc * xc, axis=-1, keepdims=True)
    return (xc * lax.rsqrt(var + EPS) * g.astype(jnp.float32) + b.astype(jnp.float32)).astype(x.dtype)


def causal_depthwise_conv(x, w, b):
    K, C = w.shape
    y = lax.conv_general_dilated(
        x, w[:, None, :], window_strides=(1,), padding=[(K - 1, 0)],
        dimension_numbers=("NWC", "WIO", "NWC"), feature_group_count=C)
    return y + b


def stick_breaking_attention(q, k, v):
    B, S, H, Dh = q.shape
    nb = S // SB_BLOCK
    scale = Dh ** -0.5
    kf = k.astype(jnp.float32)
    vf = v.astype(jnp.float32)
    qb = q.astype(jnp.float32).reshape(B, nb, SB_BLOCK, H, Dh).transpose(1, 0, 2, 3, 4)
    key_pos = jnp.arange(S)

    def block(args):
        q_blk, start = args
        z = jnp.einsum("bthd,bshd->bhts", q_blk, kf) * scale
        q_pos = start + jnp.arange(SB_BLOCK)
        mask = key_pos[None, :] < q_pos[:, None]
        log_beta = jax.nn.log_sigmoid(z)
        log_keep = jnp.where(mask, -jax.nn.softplus(z), 0.0)
        later = lax.cumsum(log_keep, axis=3, reverse=True) - log_keep
        w = jnp.where(mask, jnp.exp(log_beta + later), 0.0)
        return jnp.einsum("bhts,bshd->bthd", w, vf)

    out = lax.map(block, (qb, jnp.arange(nb, dtype=jnp.int32) * SB_BLOCK))
    return out.transpose(1, 0, 2, 3, 4).reshape(B, S, H * Dh).astype(q.dtype)


def conformer_conv(val, gate, w_dw, b_dw, ln_g, ln_b):
    h = val * jax.nn.sigmoid(gate)
    h = causal_depthwise_conv(h, w_dw, b_dw)
    h = layer_norm(h, ln_g, ln_b)
    return jax.nn.silu(h)


def chunked_spatial_gating(uv, ln_g, ln_b, w_s, b_s):
    B, S, _ = uv.shape
    u, v = jnp.split(jax.nn.gelu(uv), 2, axis=-1)
    v = layer_norm(v, ln_g, ln_b)
    nc = S // GMLP_CHUNK
    v = v.reshape(B, nc, GMLP_CHUNK, GROUP_HEADS, HEAD_DIM)
    tri = jnp.tril(jnp.ones((GMLP_CHUNK, GMLP_CHUNK), dtype=bool))
    w = jnp.where(tri, w_s, 0.0)
    mixed = jnp.einsum("hts,bcshd->bcthd", w, v) + b_s.T[:, :, None]
    return u * mixed.reshape(B, S, GROUP_WIDTH)


def rg_lru_branch(xb, gate, conv_w, conv_b, w_a, b_a, w_x, b_x, lam):
    xb = causal_depthwise_conv(xb, conv_w, conv_b)
    B, S, C = xb.shape
    xh = xb.reshape(B, S, GROUP_HEADS, HEAD_DIM)
    r = jax.nn.sigmoid(jnp.einsum("bshi,hij->bshj", xh, w_a).reshape(B, S, C) + b_a)
    i = jax.nn.sigmoid(jnp.einsum("bshi,hij->bshj", xh, w_x).reshape(B, S, C) + b_x)
    log_a = -LRU_C * r.astype(jnp.float32) * jax.nn.softplus(-lam.astype(jnp.float32))
    a = jnp.exp(log_a)
    b_in = jnp.sqrt(-jnp.expm1(2.0 * log_a)) * (i * xb).astype(jnp.float32)

    def combine(left, right):
        a1, b1 = left
        a2, b2 = right
        return a1 * a2, a2 * b1 + b2

    _, h = lax.associative_scan(combine, (a, b_in), axis=1)
    return (h * jax.nn.gelu(gate.astype(jnp.float32))).astype(xb.dtype)


def hybrid_mixer(h, w_in, q_norm_g, k_norm_g, conf_dw_w, conf_dw_b, conf_ln_g, conf_ln_b,
                 gmlp_ln_g, gmlp_ln_b, gmlp_ws, gmlp_bs, lru_conv_w, lru_conv_b,
                 lru_wa, lru_ba, lru_wx, lru_bx, lru_lambda, group_norm_g, w_out):
    B, S, _ = h.shape
    proj = jnp.einsum("bsd,dc->bsc", h, w_in)
    c1 = SB_COLS
    c2 = c1 + CONF_COLS
    c3 = c2 + GMLP_COLS
    sb, conf, gm, lru = jnp.split(proj, [c1, c2, c3], axis=-1)
    q, k, v = jnp.split(sb, 3, axis=-1)
    q = rms_norm(q.reshape(B, S, GROUP_HEADS, HEAD_DIM), q_norm_g)
    k = rms_norm(k.reshape(B, S, GROUP_HEADS, HEAD_DIM), k_norm_g)
    v = v.reshape(B, S, GROUP_HEADS, HEAD_DIM)
    y_a = stick_breaking_attention(q, k, v)
    conf_val, conf_gate = jnp.split(conf, 2, axis=-1)
    y_b = conformer_conv(conf_val, conf_gate, conf_dw_w, conf_dw_b, conf_ln_g, conf_ln_b)
    y_c = chunked_spatial_gating(gm, gmlp_ln_g, gmlp_ln_b, gmlp_ws, gmlp_bs)
    lru_x, lru_gate = jnp.split(lru, 2, axis=-1)
    y_d = rg_lru_branch(lru_x, lru_gate, lru_conv_w, lru_conv_b, lru_wa, lru_ba, lru_wx, lru_bx, lru_lambda)
    y = jnp.stack([y_a, y_b, y_c, y_d], axis=2)
    y = rms_norm(y, group_norm_g.reshape(N_GROUPS, GROUP_WIDTH)).reshape(B, S, D_MIX)
    return jnp.einsum("bsc,cd->bsd", y, w_out)


def swiglu(h, wg, wu, wd):
    return (jax.nn.silu(h @ wg) * (h @ wu)) @ wd


def top2_moe(h, router, wg, wu, wd):
    B, S, D = h.shape
    N = B * S
    A = N * TOP_K
    hf = h.reshape(N, D)
    logits = (hf @ router).astype(jnp.float32)
    top_logits, top_idx = lax.top_k(logits, TOP_K)
    gates = jax.nn.softmax(top_logits, axis=-1).astype(h.dtype)
    exp_ids = top_idx.reshape(A)
    tok_ids = jnp.repeat(jnp.arange(N, dtype=jnp.int32), TOP_K)
    gate_flat = gates.reshape(A)
    order = jnp.argsort(exp_ids)
    exp_sorted = exp_ids[order]
    tok_sorted = tok_ids[order]
    gate_sorted = gate_flat[order]
    counts = jnp.bincount(exp_ids, length=N_EXPERTS)
    starts = jnp.cumsum(counts) - counts
    padded = (counts + EXPERT_BLOCK - 1) // EXPERT_BLOCK * EXPERT_BLOCK
    pad_ends = jnp.cumsum(padded)
    pad_starts = pad_ends - padded
    rank = jnp.arange(A, dtype=jnp.int32) - starts[exp_sorted]
    dest = pad_starts[exp_sorted] + rank
    n_blocks = -(-A // EXPERT_BLOCK) + N_EXPERTS
    n_rows = n_blocks * EXPERT_BLOCK
    row_tok = jnp.full((n_rows,), N, dtype=jnp.int32).at[dest].set(tok_sorted)
    row_gate = jnp.zeros((n_rows,), h.dtype).at[dest].set(gate_sorted)
    block_start = jnp.arange(n_blocks, dtype=jnp.int32) * EXPERT_BLOCK
    block_exp = jnp.minimum(jnp.searchsorted(pad_ends, block_start, side="right"), N_EXPERTS - 1)
    h_pad = jnp.concatenate([hf, jnp.zeros((1, D), hf.dtype)], axis=0)

    def expert_block(args):
        tok, gate, e = args
        y = swiglu(h_pad[tok], wg[e], wu[e], wd[e])
        return y * gate[:, None]

    ys = lax.map(expert_block, (row_tok.reshape(n_blocks, EXPERT_BLOCK),
                                row_gate.reshape(n_blocks, EXPERT_BLOCK), block_exp))
    out = jnp.zeros((N + 1, D), h.dtype).at[row_tok].add(ys.reshape(n_rows, D))
    return out[:N].reshape(B, S, D)


def setup_inputs(seed: int = 0) -> dict:
    key = jax.random.key(seed)
    ks = jax.random.split(key, 40)
    f32 = jnp.float32

    def nrm(k, shape, scale):
        return jax.random.normal(k, shape, f32) * scale

    def gain(k, shape):
        return 1.0 + 0.02 * jax.random.normal(k, shape, f32)

    def bias(k, shape):
        return 0.02 * jax.random.normal(k, shape, f32)

    u = jax.random.uniform(ks[20], (DEPTH, GROUP_WIDTH), f32, minval=0.9, maxval=0.999)
    s = u ** (1.0 / LRU_C)
    lru_lambda = jnp.log(s) - jnp.log1p(-s)
    return {
        "x": jax.random.normal(ks[0], (BATCH, SEQ, D_MODEL), f32),
        "norm1_g": gain(ks[1], (DEPTH, D_MODEL)),
        "w_in": nrm(ks[2], (DEPTH, D_MODEL, IN_COLS), D_MODEL ** -0.5),
        "q_norm_g": gain(ks[3], (DEPTH, HEAD_DIM)),
        "k_norm_g": gain(ks[4], (DEPTH, HEAD_DIM)),
        "conf_dw_w": nrm(ks[5], (DEPTH, CONF_KERNEL, GROUP_WIDTH), CONF_KERNEL ** -0.5),
        "conf_dw_b": bias(ks[6], (DEPTH, GROUP_WIDTH)),
        "conf_ln_g": gain(ks[7], (DEPTH, GROUP_WIDTH)),
        "conf_ln_b": bias(ks[8], (DEPTH, GROUP_WIDTH)),
        "gmlp_ln_g": gain(ks[9], (DEPTH, GROUP_WIDTH)),
        "gmlp_ln_b": bias(ks[10], (DEPTH, GROUP_WIDTH)),
        "gmlp_ws": nrm(ks[11], (DEPTH, GROUP_HEADS, GMLP_CHUNK, GMLP_CHUNK), GMLP_CHUNK ** -0.5),
        "gmlp_bs": gain(ks[12], (DEPTH, GROUP_HEADS, GMLP_CHUNK)),
        "lru_conv_w": nrm(ks[13], (DEPTH, LRU_CONV, GROUP_WIDTH), LRU_CONV ** -0.5),
        "lru_conv_b": bias(ks[14], (DEPTH, GROUP_WIDTH)),
        "lru_wa": nrm(ks[15], (DEPTH, GROUP_HEADS, HEAD_DIM, HEAD_DIM), HEAD_DIM ** -0.5),
        "lru_ba": bias(ks[16], (DEPTH, GROUP_WIDTH)),
        "lru_wx": nrm(ks[17], (DEPTH, GROUP_HEADS, HEAD_DIM, HEAD_DIM), HEAD_DIM ** -0.5),
        "lru_bx": bias(ks[18], (DEPTH, GROUP_WIDTH)),
        "lru_lambda": lru_lambda,
        "group_norm_g": gain(ks[21], (DEPTH, D_MIX)),
        "w_out": nrm(ks[22], (DEPTH, D_MIX, D_MODEL), D_MIX ** -0.5),
        "norm2_g": gain(ks[23], (DEPTH, D_MODEL)),
        "ffn_w_gate": nrm(ks[24], (N_DENSE, D_MODEL, D_FF), D_MODEL ** -0.5),
        "ffn_w_up": nrm(ks[25], (N_DENSE, D_MODEL, D_FF), D_MODEL ** -0.5),
        "ffn_w_down": nrm(ks[26], (N_DENSE, D_FF, D_MODEL), D_FF ** -0.5),
        "moe_router": nrm(ks[27], (N_MOE, D_MODEL, N_EXPERTS), D_MODEL ** -0.5),
        "moe_w_gate": nrm(ks[28], (N_MOE, N_EXPERTS, D_MODEL, D_FF_EXPERT), D_MODEL ** -0.5),
        "moe_w_up": nrm(ks[29], (N_MOE, N_EXPERTS, D_MODEL, D_FF_EXPERT), D_MODEL ** -0.5),
        "moe_w_down": nrm(ks[30], (N_MOE, N_EXPERTS, D_FF_EXPERT, D_MODEL), D_FF_EXPERT ** -0.5),
    }


def reference(x, norm1_g, w_in, q_norm_g, k_norm_g, conf_dw_w, conf_dw_b, conf_ln_g, conf_ln_b,
              gmlp_ln_g, gmlp_ln_b, gmlp_ws, gmlp_bs, lru_conv_w, lru_conv_b, lru_wa, lru_ba,
              lru_wx, lru_bx, lru_lambda, group_norm_g, w_out, norm2_g,
              ffn_w_gate, ffn_w_up, ffn_w_down, moe_router, moe_w_gate, moe_w_up, moe_w_down):
    for l in range(DEPTH):
        h = rms_norm(x, norm1_g[l])
        x = x + hybrid_mixer(h, w_in[l], q_norm_g[l], k_norm_g[l], conf_dw_w[l], conf_dw_b[l],
                             conf_ln_g[l], conf_ln_b[l], gmlp_ln_g[l], gmlp_ln_b[l], gmlp_ws[l],
                             gmlp_bs[l], lru_conv_w[l], lru_conv_b[l], lru_wa[l], lru_ba[l],
                             lru_wx[l], lru_bx[l], lru_lambda[l], group_norm_g[l], w_out[l])
        h = rms_norm(x, norm2_g[l])
        if l % 2 == 0:
            j = l // 2
            x = x + swiglu(h, ffn_w_gate[j], ffn_w_up[j], ffn_w_down[j])
        else:
            j = l // 2
            x = x + top2_moe(h, moe_router[j], moe_w_gate[j], moe_w_up[j], moe_w_down[j])
    return x
```

```python
import numpy as np
from contextlib import ExitStack
import concourse.bass as bass
import concourse.mybir as mybir
from concourse.bass_utils import run_bass_kernel_spmd

F32 = mybir.dt.float32
BF16 = mybir.dt.bfloat16
I32 = mybir.dt.int32
F32R = mybir.dt.float32r
AF = mybir.ActivationFunctionType
ALU = mybir.AluOpType
AX = mybir.AxisListType

D = 1024
S = 2048
NT = 16
L = 2
INC = 2304
DFF = 2816
NE = 8
DFE = 3584
EPS = 1e-6
NPC = 110
N_CORES = 8


class Prog:
    def __init__(self, nc):
        self.nc = nc
        self.ops = []
        self.last_w = {}
        self.readers = {}
        self.dma_cnt = {}
        self.dma_last = {}
        self.dma_arena = {}
        self.fence_deps = {}
        self.fence_pending = set()

    def _deps(self, r, w):
        deps = {}
        for x in r:
            lw = self.last_w.get(x)
            if lw is not None:
                deps[lw] = True
        for x in w:
            lw = self.last_w.get(x)
            if lw is not None:
                deps.setdefault(lw, False)
            for rd in self.readers.get(x, ()):
                deps.setdefault(rd, False)
        i = len(self.ops)
        for x in r:
            self.readers.setdefault(x, []).append(i)
        for x in w:
            self.last_w[x] = i
            self.readers[x] = []
        return deps

    @staticmethod
    def _persistent(name):
        return name.startswith(("X", "HT", "YT", "WS", "ps"))

    def _fence_check(self, eng, r, w, deps):
        arena = any(not self._persistent(x) for x in r) or any(not self._persistent(x) for x in w)
        if arena and eng in self.fence_pending:
            self.fence_pending.discard(eng)
            for i in self.fence_deps:
                deps[i] = True
        return arena

    def fence(self):
        last = {}
        for i, o in enumerate(self.ops):
            if o["dma"] is None and o["fn"] is not None:
                last[o["eng"]] = i
        deps = {i: True for i in last.values()}
        for k, i in self.dma_last.items():
            if self.dma_arena.get(k):
                deps[i] = True
        self.fence_deps = deps
        self.fence_pending = set(["pe", "act", "dve", "pool", "sp"])

    def op(self, eng, fn, r=(), w=()):
        deps = self._deps(r, w)
        self._fence_check(eng, r, w, deps)
        self.ops.append(dict(eng=eng, fn=fn, deps=deps, dma=None))
        return len(self.ops) - 1

    def dma(self, out, in_, key, r=(), w=(), eng="sp"):
        deps = self._deps(r, w)
        self.dma_arena[key] = self._fence_check(eng, r, w, deps)
        self.dma_cnt[key] = self.dma_cnt.get(key, 0) + 16
        fn = lambda e: e.dma_start(out=out, in_=in_)
        self.ops.append(dict(eng=eng, fn=fn, deps=deps, dma=(key, self.dma_cnt[key])))
        self.dma_last[key] = len(self.ops) - 1
        return len(self.ops) - 1

    def wait_all(self, eng, idxs):
        self.ops.append(dict(eng=eng, fn=None, deps={i: True for i in idxs}, dma=None))

    def barrier(self):
        last = {}
        for i, o in enumerate(self.ops):
            if o["dma"] is None and o["fn"] is not None:
                last[o["eng"]] = i
        deps = {i: True for i in last.values()}
        for i in self.dma_last.values():
            deps[i] = True
        for eng in ("pe", "act", "dve", "pool", "sp"):
            self.ops.append(dict(eng=eng, fn=None, deps=dict(deps), dma=None, bar=True))
        self.fence_deps = {}
        self.fence_pending = set()

    def emit(self, stack):
        nc = self.nc
        ops = self.ops
        for o in ops:
            o["sig"] = False
        for o in ops:
            latest = {}
            for j, raw in o["deps"].items():
                p = ops[j]
                if p["dma"] is not None:
                    continue
                if p["eng"] == o["eng"]:
                    if o["eng"] == "pe" or o.get("bar"):
                        continue
                if j > latest.get(p["eng"], -1):
                    latest[p["eng"]] = j
            o["edeps"] = latest
            for j in latest.values():
                ops[j]["sig"] = True
        cnt = {}
        for o in ops:
            if o["dma"] is None and o["sig"]:
                cnt[o["eng"]] = cnt.get(o["eng"], 0) + 1
                o["sigval"] = cnt[o["eng"]]
        engs = ["pe", "act", "dve", "pool", "sp"]
        sems = {e: stack.enter_context(nc.semaphore("s_" + e)) for e in engs}
        dsems = {k: stack.enter_context(nc.semaphore("d_%d" % n)) for n, k in enumerate(self.dma_cnt)}
        block = stack.enter_context(nc.Block())

        def run(ename, eng):
            waited = {}
            for o in ops:
                if o["eng"] != ename:
                    continue
                wl = []
                for j, raw in o["deps"].items():
                    p = ops[j]
                    if p["dma"] is not None:
                        key = p["dma"][0]
                        wl.append((dsems[key], p["dma"][1], "d" + str(key)))
                for pe_, j in o["edeps"].items():
                    wl.append((sems[pe_], ops[j]["sigval"], pe_))
                for s, v, wk in wl:
                    if waited.get(wk, 0) >= v:
                        continue
                    waited[wk] = v
                    eng.wait_ge(s, v)
                if o["fn"] is None:
                    continue
                ins = o["fn"](eng)
                if o["dma"] is not None:
                    ins.then_inc(dsems[o["dma"][0]], 16)
                elif o["sig"]:
                    ins.then_inc(sems[ename], 1)

        @block.tensor
        def _(e):
            run("pe", e)

        @block.scalar
        def _(e):
            run("act", e)

        @block.vector
        def _(e):
            run("dve", e)

        @block.gpsimd
        def _(e):
            run("pool", e)

        @block.sync
        def _(e):
            run("sp", e)


class Res:
    def __init__(self, ap, name, names=None):
        self.ap = ap
        self.name = name
        self.names = names if names is not None else [name]


def build_nc(nseq=2, n_phases=None, layers=(0, 1)):
    nc = bass.Bass("TRN2", target_bir_lowering=False)

    def din(name, shape):
        return nc.dram_tensor(name, list(shape), F32, kind="ExternalInput").ap()

    x_d = din("x", [nseq, S, D])
    w_in_d = din("w_in", [L, D, INC])
    w_out_d = din("w_out", [L, D, D])
    fg_d = din("ffn_w_gate", [1, D, DFF])
    fu_d = din("ffn_w_up", [1, D, DFF])
    fd_d = din("ffn_w_down", [1, DFF, D])
    mg_d = din("moe_w_gate", [1, NE, D, DFE])
    mu_d = din("moe_w_up", [1, NE, D, DFE])
    md_d = din("moe_w_down", [1, NE, DFE, D])
    pcol_d = din("pcol", [128, L * NPC])
    pbc_d = din("pbc", [L, 1, 1536])
    wsT_d = din("wsT", [L, 128, 512])
    bsT_d = din("bsT", [L, 128, 256])
    routerF_d = din("routerF", [128, 8 * NE])
    wa_d = din("lru_wa", [L, 4, 64, 64])
    wx_d = din("lru_wx", [L, 4, 64, 64])
    y_d = nc.dram_tensor("y", [nseq, S, D], F32, kind="ExternalOutput").ap()

    with ExitStack() as st:
        P = Prog(nc)

        def sb(name, shape, dt=F32):
            return st.enter_context(nc.sbuf_tensor(name, shape, dt))

        X = sb("X", [128, NT, D])
        HT = sb("HT", [128, 8, S], BF16)
        YT = sb("YT", [128, 2, S], BF16)
        WS = [sb("WS%d" % i, [128, 6144], BF16) for i in range(2)]
        SCR = sb("SCR", [128, 12288])
        GNS = SCR[:, 10240:12288]
        SPR = sb("SPR", [128, 10 * 512], F32R)
        identb = sb("identb", [128, 128], BF16)
        identf = sb("identf", [128, 128])
        onesf = sb("onesf", [128, 128])
        onesb = sb("onesb", [128, 128], BF16)
        blkones = sb("blkones", [128, 128])
        ntri = sb("ntri", [128, 128], F32R)
        nones = sb("nones", [128, 128], F32R)
        MW = sb("MW", [128, 896])
        NEGW = sb("NEGW", [128, 896], BF16)
        maskle = sb("maskle", [128, 128])
        pcol = sb("pcol_s", [128, L * NPC])
        small = sb("small", [128, 512])
        PS = [st.enter_context(nc.psum_tensor("ps%d" % i, [128, 512], F32)) for i in range(8)]

        ss = small[:, 0:16]
        lnt = small[:, 16:32]
        rs = small[:, 32:48]
        qk8 = small[:, 48:52]
        clc = small[:, 52:60]
        logits = small[:, 64:192].rearrange("p (t e) -> p t e", e=NE)
        gates = small[:, 192:320].rearrange("p (t e) -> p t e", e=NE)
        tk = small[:, 320:400]
        bnst = small[:, 400:420]

        cnt = {"ps": 0, "psl": 0, "ws": 0}

        def psn():
            i = cnt["ps"] % 6
            cnt["ps"] += 1
            return Res(PS[i][:], "ps%d" % i)

        def psl():
            i = 6 + cnt["psl"] % 2
            cnt["psl"] += 1
            return Res(PS[i][:], "ps%d" % i)

        def ws_slot(i):
            return Res(WS[i][:], "WS%du" % i, ["WS%du" % i, "WS%dd" % i])

        def ws_next():
            i = cnt["ws"] % 2
            cnt["ws"] += 1
            return ws_slot(i)

        class Arena:
            def __init__(self, ap):
                self.ap = ap
                self.off = 0

            def reset(self):
                self.off = 0

            def f32(self, n):
                a = self.ap[:, self.off:self.off + n]
                self.off += n
                assert self.off <= self.ap.shape[1], self.off
                return a

            def bf16(self, n):
                assert n % 2 == 0
                return self.f32(n // 2).bitcast(BF16)

        A = Arena(SCR[:])

        def MM(out, lhsT, rhs, start, stop, r, w):
            P.op("pe", lambda e: e.matmul(out, lhsT=lhsT, rhs=rhs, start=start, stop=stop), r=r, w=w)

        def TR(out, in_, r, w):
            P.op("pe", lambda e: e.transpose(out=out, in_=in_, identity=identb[:]), r=r, w=w)

        def ACT(out, in_, func, r, w, **kw):
            P.op("act", lambda e: e.activation(out=out, in_=in_, func=func, **kw), r=r, w=w)

        def TT(out, in0, in1, op, r, w, eng="dve"):
            P.op(eng, lambda e: e.tensor_tensor(out=out, in0=in0, in1=in1, op=op), r=r, w=w)

        def TS(out, in0, s1, s2, op0, op1, r, w, eng="dve"):
            if s2 is None:
                P.op(eng, lambda e: e.tensor_scalar(out=out, in0=in0, scalar1=s1, scalar2=None, op0=op0), r=r, w=w)
            else:
                P.op(eng, lambda e: e.tensor_scalar(out=out, in0=in0, scalar1=s1, scalar2=s2, op0=op0, op1=op1), r=r, w=w)

        def STT(out, in0, scalar, in1, op0, op1, r, w, **kw):
            P.op("dve", lambda e: e.scalar_tensor_tensor(out=out, in0=in0, scalar=scalar, in1=in1, op0=op0, op1=op1, **kw), r=r, w=w)

        def CP(out, in_, r, w, eng="dve"):
            P.op(eng, lambda e: e.tensor_copy(out=out, in_=in_), r=r, w=w)

        def MS(ap, val, w, eng="dve"):
            P.op(eng, lambda e: e.memset(ap, val), w=w)

        def pc(l, col, n=1):
            return pcol[:, l * NPC + col: l * NPC + col + n]

        io = SCR[:, 0:896].bitcast(I32)
        iof = SCR[:, 896:1792]
        P.op("pool", lambda e: e.iota(io, pattern=[[1, 896]], base=0, channel_multiplier=-1), w=["io"])
        CP(iof, io, r=["io"], w=["iof"])
        P.op("dve", lambda e: e.tensor_single_scalar(out=MW[:], in_=iof, scalar=384.0, op=ALU.is_gt), r=["iof"], w=["cMW"])
        P.op("dve", lambda e: e.tensor_scalar(out=NEGW[:], in0=MW[:], scalar1=30000.0, scalar2=-30000.0, op0=ALU.mult, op1=ALU.add), r=["cMW"], w=["c"])
        P.op("dve", lambda e: e.tensor_single_scalar(out=identf[:], in_=iof[:, 0:128], scalar=0.0, op=ALU.is_equal), r=["iof"], w=["c"])
        P.op("dve", lambda e: e.tensor_single_scalar(out=identb[:], in_=iof[:, 0:128], scalar=0.0, op=ALU.is_equal), r=["iof"], w=["c"])
        P.op("dve", lambda e: e.tensor_single_scalar(out=maskle[:], in_=iof[:, 0:128], scalar=0.0, op=ALU.is_ge), r=["iof"], w=["c"])
        P.op("dve", lambda e: e.tensor_scalar(out=ntri[:], in0=iof[:, 0:128], scalar1=0.0, scalar2=-1.0, op0=ALU.is_le, op1=ALU.mult), r=["iof"], w=["c"])
        MS(onesf[:], 1.0, w=["c"])
        MS(onesb[:], 1.0, w=["c"])
        P.op("dve", lambda e: e.tensor_scalar(out=nones[:], in0=iof[:, 0:128], scalar1=0.0, scalar2=-1.0, op0=ALU.mult, op1=ALU.add), r=["iof"], w=["c"])
        MS(blkones[:], 0.0, w=["c"])
        MS(blkones[0:64, 0:64], 1.0, w=["c"])
        MS(blkones[64:128, 64:128], 1.0, w=["c"])
        P.dma(pcol[:], pcol_d, "pcol", w=["pcol"])
        for l in range(L):
            TS(qk8[:, l:l + 1], pc(l, 38), 0.125, None, ALU.mult, None, r=["pcol"], w=["c"])
        P.barrier()

        def phase_load(sq):
            yield
            xs = x_d[sq].rearrange("(t p) d -> p t d", p=128)
            for g in range(4):
                P.dma(X[:, 4 * g:4 * g + 4, :], xs[:, 4 * g:4 * g + 4, :], "xin%d" % g, w=["X%d" % t for t in range(4 * g, 4 * g + 4)])

        stores = []

        def phase_store(sq):
            yield
            ys = y_d[sq].rearrange("(t p) d -> p t d", p=128)
            for g in range(4):
                stores.append(P.dma(ys[:, 4 * g:4 * g + 4, :], X[:, 4 * g:4 * g + 4, :], "xout%d" % g, r=["X%d" % t for t in range(4 * g, 4 * g + 4)]))

        def phase_norm(l, gcol0):
            yield
            A.reset()
            hss = [A.bf16(4096).rearrange("p (i d) -> p i d", i=4) for _ in range(2)]
            junk = A.bf16(1024)

            def N1(g):
                hs = hss[g % 2]
                for i in range(4):
                    t = 4 * g + i
                    ACT(junk, X[:, t, :], AF.Square, r=["X%d" % t], w=["junk", "ss"], accum_out=ss[:, t:t + 1])
                ACT(lnt[:, 4 * g:4 * g + 4], ss[:, 4 * g:4 * g + 4], AF.Ln, r=["ss"], w=["lnt"], scale=1.0 / D, bias=EPS)
                ACT(rs[:, 4 * g:4 * g + 4], lnt[:, 4 * g:4 * g + 4], AF.Exp, r=["lnt"], w=["rs"], scale=-0.5)
                for i in range(4):
                    t = 4 * g + i
                    TS(hs[:, i, :], X[:, t, :], rs[:, t:t + 1], None, ALU.mult, None, r=["X%d" % t, "rs"], w=["hs%d_%d" % (g % 2, i)])

            def N2(g):
                hs = hss[g % 2]
                for c in range(8):
                    pb = psn()
                    ptb = pb.ap.bitcast(BF16)
                    for i in range(4):
                        TR(ptb[:, 128 * i:128 * i + 128], hs[:, i, 128 * c:128 * c + 128], r=["hs%d_%d" % (g % 2, i)], w=[pb.name])
                    dst = HT[:, c, 512 * g:512 * g + 512]
                    if c % 2 == 0:
                        ACT(dst, ptb[:, 0:512], AF.Identity, r=[pb.name], w=["HT%d" % g], scale=pc(l, gcol0 + c))
                    else:
                        TS(dst, ptb[:, 0:512], pc(l, gcol0 + c), None, ALU.mult, None, r=[pb.name], w=["HT%d" % g])

            N1(0)
            for g in range(4):
                if g + 1 < 4:
                    N1(g + 1)
                N2(g)

        def w_in_view(l):
            return w_in_d[l].rearrange("(c p) n -> p c n", p=128)

        def group_norm_out(l, gi):
            slot = ws_next()
            w3 = slot.ap[:, 0:2048].rearrange("p (c n) -> p c n", c=2)
            P.dma(w3, w_out_d[l][256 * gi:256 * gi + 256, :].rearrange("(c p) n -> p c n", p=128), slot.name,
                  w=slot.names, eng="pool")
            sqb = [GNS[:, 256 * i:256 * i + 256].bitcast(BF16) for i in range(4)]
            ln_ = GNS[:, 1024:1536]
            rr = GNS[:, 1536:2048]
            for g in range(4):
                sl = slice(512 * g, 512 * g + 512)
                sq0 = sqb[2 * (g % 2)]
                sq1 = sqb[2 * (g % 2) + 1]
                n0 = "gsq%d" % (2 * (g % 2))
                n1 = "gsq%d" % (2 * (g % 2) + 1)
                ACT(sq0, YT[:, 0, sl], AF.Square, r=["YT0_%d" % g], w=[n0])
                ACT(sq1, YT[:, 1, sl], AF.Square, r=["YT1_%d" % g], w=[n1])
                pb = psn()
                MM(pb.ap, onesb[:], sq0, True, False, r=[n0], w=[pb.name])
                MM(pb.ap, onesb[:], sq1, False, True, r=[n1], w=[pb.name])
                ACT(ln_, pb.ap, AF.Ln, r=[pb.name], w=["gln"], scale=1.0 / 256, bias=EPS)
                ACT(rr, ln_, AF.Exp, r=["gln"], w=["grr"], scale=-0.5)
                for c in range(2):
                    STT(YT[:, c, sl], YT[:, c, sl], pc(l, 16 + 2 * gi + c), rr, ALU.mult, ALU.mult,
                        r=["YT%d_%d" % (c, g), "grr"], w=["YT%d_%d" % (c, g)])
            for g in range(4):
                for i in range(4):
                    t = 4 * g + i
                    for h in range(2):
                        py = psn()
                        for c in range(2):
                            MM(py.ap, YT[:, c, 128 * t:128 * t + 128], w3[:, c, 512 * h:512 * h + 512], c == 0, c == 1,
                               r=["YT%d_%d" % (c, g)] + slot.names, w=[py.name])
                        xs_ = X[:, t, 512 * h:512 * h + 512]
                        TT(xs_, py.ap, xs_, ALU.add, r=[py.name, "X%d" % t], w=["X%d" % t])

        def phase_attn(l, pr):
            slot = ws_next()
            wv3 = slot.ap[:, 0:3072].rearrange("p (c n) -> p c n", c=8)
            for j, c0 in enumerate([128 * pr, 256 + 128 * pr, 512 + 128 * pr]):
                P.dma(wv3[:, :, 128 * j:128 * j + 128], w_in_view(l)[:, :, c0:c0 + 128], slot.name, w=slot.names, eng="pool")
            yield
            A.reset()
            QTh = [A.bf16(2048) for _ in range(2)]
            KT = A.bf16(2048)
            Vpf = A.bf16(4096)
            Vp = Vpf.rearrange("p (s h n) -> p s h n", s=16, h=2)
            eb = [A.f32(512) for _ in range(5)]
            spb = [SPR[:, 512 * i:512 * i + 512].bitcast(F32) for i in range(5)]
            Rb = [SPR[:, 512 * i:512 * i + 512].bitcast(F32) for i in range(5, 10)]
            wb = [A.bf16(512) for _ in range(4)]
            sq = A.f32(512)
            lnv = A.f32(512)
            rr = A.f32(512)
            assert A.off <= 10240
            MS(Vpf, 0.0, w=["Vp%d" % g for g in range(4)])
            MS(QTh[0][64:128, :], 0.0, w=["QTz"])
            MS(QTh[1][0:64, :], 0.0, w=["QTz"])
            for g in range(4):
                sl = slice(512 * g, 512 * g + 512)
                for wi in range(2):
                    pa = psn()
                    for dc in range(8):
                        MM(pa.ap, wv3[:, dc, 128 * wi:128 * wi + 128], HT[:, dc, sl], dc == 0, dc == 7,
                           r=slot.names + ["HT%d" % g], w=[pa.name])
                    ACT(sq, pa.ap, AF.Square, r=[pa.name], w=["asq"])
                    pk = psn()
                    MM(pk.ap, blkones[:], sq, True, True, r=["asq"], w=[pk.name])
                    ACT(lnv, pk.ap, AF.Ln, r=[pk.name], w=["alnv"], scale=1.0 / 64, bias=EPS)
                    ACT(rr, lnv, AF.Exp, r=["alnv"], w=["arr"], scale=-0.5)
                    if wi == 0:
                        for h in range(2):
                            hp = slice(64 * h, 64 * h + 64)
                            STT(QTh[h][hp, sl], pa.ap[hp, :], qk8[hp, l:l + 1], rr[hp, :], ALU.mult, ALU.mult,
                                r=[pa.name, "arr", "QTz"], w=["QT%d" % g])
                    else:
                        STT(KT[:, sl], pa.ap, pc(l, 39), rr, ALU.mult, ALU.mult, r=[pa.name, "arr"], w=["KT%d" % g])
                pv = psn()
                for i in range(4):
                    t = 4 * g + i
                    for dc in range(8):
                        MM(pv.ap[:, 128 * i:128 * i + 128], HT[:, dc, 128 * t:128 * t + 128], wv3[:, dc, 256:384],
                           dc == 0, dc == 7, r=slot.names + ["HT%d" % g], w=[pv.name])
                pv3 = pv.ap.rearrange("p (i n) -> p i n", i=4)
                ACT(Vp[:, 4 * g:4 * g + 4, 0, 0:64], pv3[:, :, 0:64], AF.Copy, r=[pv.name], w=["Vp%d" % g])
                CP(Vp[:, 4 * g:4 * g + 4, 1, 64:128], pv3[:, :, 64:128], r=[pv.name], w=["Vp%d" % g])
            blocks = []
            for qt in range(4):
                for h in range(2):
                    jmax = 4 * qt + 3
                    for j in range(jmax, -1, -1):
                        blocks.append(dict(qt=qt, h=h, j=j, k=j - 4 * qt, pos=jmax - j,
                                           first=(h == 0 and j == jmax), last=(h == 1 and j == 0)))
            for n, b in enumerate(blocks):
                b["n"] = n
            pos = {}

            def zmm(pt, b, stop_):
                qs = slice(512 * b["qt"], 512 * b["qt"] + 512)
                ks = slice(128 * b["j"], 128 * b["j"] + 128)
                rq = ["KT%d" % (b["j"] // 4), "QT%d" % b["qt"]]
                k = b["k"]
                MM(pt.ap, KT[:, ks], QTh[b["h"]][:, qs], True, stop_ and k < 0, r=rq, w=[pt.name])
                if k >= 0:
                    MM(pt.ap, identb[:], NEGW[:, 384 - 128 * k:384 - 128 * k + 512], False, stop_, r=[], w=[pt.name])

            def S1(b):
                n = b["n"]
                pz = psn()
                zmm(pz, b, True)
                e_ = eb[n % 5]
                en = "ae%d" % (n % 5)
                ACT(e_, pz.ap, AF.Exp, r=[pz.name], w=[en])
                sp = spb[n % 5]
                spn = "sp%d" % (n % 5)
                ACT(sp.bitcast(F32R), e_, AF.Ln, r=[en], w=[spn], bias=1.0)
                if b["pos"] == 0:
                    b["R"] = None
                else:
                    prev = blocks[n - 1]
                    psp = spb[(n - 1) % 5]
                    pspn = "sp%d" % ((n - 1) % 5)
                    nr = Rb[n % 5]
                    nrn = "aR%d" % (n % 5)
                    if prev["R"] is None:
                        P.op("dve", lambda e: e.tensor_copy(out=nr.bitcast(F32R), in_=psp), r=[pspn], w=[nrn])
                    else:
                        TT(nr.bitcast(F32R), prev["R"].ap, psp, ALU.add, r=[prev["R"].name, pspn], w=[nrn])
                    b["R"] = Res(nr, nrn)

            def S2(b):
                n = b["n"]
                sp = spb[n % 5]
                spn = "sp%d" % (n % 5)
                R = b["R"]
                pb = psn()
                zmm(pb, b, False)
                MM(pb.ap, ntri[:], sp.bitcast(F32R), False, R is None, r=[spn], w=[pb.name])
                if R is not None:
                    MM(pb.ap, nones[:], R.ap.bitcast(F32R), False, True, r=[R.name], w=[pb.name])
                w_ = wb[n % 4]
                wn = "aw%d" % (n % 4)
                ACT(w_, pb.ap, AF.Exp, r=[pb.name], w=[wn])

            def S3(b):
                n = b["n"]
                qs = slice(512 * b["qt"], 512 * b["qt"] + 512)
                if b["first"]:
                    pos["po"] = psl()
                po = pos["po"]
                w_ = wb[n % 4]
                wn = "aw%d" % (n % 4)
                MM(po.ap, Vp[:, b["j"], b["h"], :], w_, b["first"], b["last"], r=["Vp%d" % (b["j"] // 4), wn], w=[po.name])
                if b["last"]:
                    ACT(YT[:, pr, qs], po.ap, AF.Copy, r=[po.name], w=["YT%d_%d" % (pr, b["qt"])])

            LOOK = 2
            for i in range(len(blocks) + 2 * LOOK):
                if i < len(blocks):
                    S1(blocks[i])
                if 0 <= i - LOOK < len(blocks):
                    S2(blocks[i - LOOK])
                if 0 <= i - 2 * LOOK < len(blocks):
                    S3(blocks[i - 2 * LOOK])
            if pr == 1:
                P.fence()
                group_norm_out(l, 0)

        def phase_conf(l):
            slot = ws_next()
            wv3 = slot.ap[:, 0:4096].rearrange("p (c n) -> p c n", c=8)
            P.dma(wv3, w_in_view(l)[:, :, 768:1280], slot.name, w=slot.names, eng="pool")
            yield
            A.reset()
            hpad = A.bf16(4160).rearrange("p (c n) -> p c n", c=2)
            dg = A.bf16(7936).rearrange("p (c k n) -> p c k n", c=2, k=31)
            sig = A.f32(512)
            cv = A.f32(1024).rearrange("p (c n) -> p c n", c=2)
            sqv = A.f32(1024).rearrange("p (c n) -> p c n", c=2)
            mean = A.f32(512)
            tmp = A.f32(512)
            rr = A.f32(512)
            for c in range(2):
                MS(hpad[:, c, 0:30], 0.0, w=["hp%d" % c])
                for k in range(31):
                    TS(dg[:, c, k, :], identf[:], pc(l, 40 + 31 * c + k), None, ALU.mult, None, r=[], w=["dg"])
            for g in range(4):
                sl = slice(512 * g, 512 * g + 512)
                for c in range(2):
                    pv = psn()
                    pg = psn()
                    for dc in range(8):
                        MM(pv.ap, wv3[:, dc, 128 * c:128 * c + 128], HT[:, dc, sl], dc == 0, dc == 7, r=slot.names + ["HT%d" % g], w=[pv.name])
                    for dc in range(8):
                        MM(pg.ap, wv3[:, dc, 256 + 128 * c:256 + 128 * c + 128], HT[:, dc, sl], dc == 0, dc == 7, r=slot.names + ["HT%d" % g], w=[pg.name])
                    ACT(sig, pg.ap, AF.Sigmoid, r=[pg.name], w=["csig"])
                    TT(hpad[:, c, 30 + 512 * g:30 + 512 * g + 512], pv.ap, sig, ALU.mult, r=[pv.name, "csig"], w=["hp%d" % c])
            for g in range(4):
                sl = slice(512 * g, 512 * g + 512)
                for c in range(2):
                    pcv = psn()
                    for k in range(31):
                        MM(pcv.ap, dg[:, c, k, :], hpad[:, c, 512 * g + k:512 * g + k + 512], k == 0, k == 30, r=["dg", "hp%d" % c], w=[pcv.name])
                    ACT(cv[:, c, :], pcv.ap, AF.Identity, r=[pcv.name], w=["cv%d" % c], bias=pc(l, 24 + c))
                    ACT(sqv[:, c, :], cv[:, c, :], AF.Square, r=["cv%d" % c], w=["csq%d" % c])
                p1 = psn()
                p2 = psn()
                for c in range(2):
                    MM(p1.ap, onesf[:], cv[:, c, :], c == 0, c == 1, r=["cv%d" % c], w=[p1.name])
                for c in range(2):
                    MM(p2.ap, onesf[:], sqv[:, c, :], c == 0, c == 1, r=["csq%d" % c], w=[p2.name])
                TS(mean, p1.ap, 1.0 / 256, None, ALU.mult, None, r=[p1.name], w=["cmean"])
                TT(tmp, mean, mean, ALU.mult, r=["cmean"], w=["ctmp"])
                STT(tmp, p2.ap, 1.0 / 256, tmp, ALU.mult, ALU.subtract, r=[p2.name, "ctmp"], w=["ctmp"])
                ACT(tmp, tmp, AF.Ln, r=["ctmp"], w=["ctmp"], bias=EPS)
                ACT(rr, tmp, AF.Exp, r=["ctmp"], w=["crr"], scale=-0.5)
                for c in range(2):
                    TT(cv[:, c, :], cv[:, c, :], mean, ALU.subtract, r=["cv%d" % c, "cmean"], w=["cv%d" % c])
                    TT(cv[:, c, :], cv[:, c, :], rr, ALU.mult, r=["cv%d" % c, "crr"], w=["cv%d" % c])
                    ACT(YT[:, c, sl], cv[:, c, :], AF.Silu, r=["cv%d" % c], w=["YT%d_%d" % (c, g)],
                        scale=pc(l, 26 + c), bias=pc(l, 28 + c))
            group_norm_out(l, 1)

        def phase_gmlp(l):
            slot = ws_next()
            wv3 = slot.ap[:, 0:4096].rearrange("p (c n) -> p c n", c=8)
            P.dma(wv3, w_in_view(l)[:, :, 1280:1792], slot.name, w=slot.names, eng="pool")
            yield
            A.reset()
            uT = A.bf16(4096).rearrange("p (c n) -> p c n", c=2)
            vpf = A.bf16(8192)
            vp = vpf.rearrange("p (s h n) -> p s h n", s=16, h=4)
            WTr = A.bf16(512).rearrange("p (h t) -> p h t", h=4)
            WTm = A.bf16(512).rearrange("p (h t) -> p h t", h=4)
            bsT = A.f32(256).rearrange("p (c t) -> p c t", c=2)
            bs4 = A.f32(1024).rearrange("p (c q t) -> p c q t", c=2, q=4)
            gb = A.f32(512)
            gvb = [A.f32(256) for _ in range(2)]
            vnb = [A.f32(256) for _ in range(2)]
            tmp = A.f32(512)
            P.dma(WTr, wsT_d[l].rearrange("p (h t) -> p h t", h=4), "gWT", w=["gWTr"], eng="pool")
            P.dma(bsT, bsT_d[l].rearrange("p (c t) -> p c t", c=2), "gbs", w=["gbsT"])
            P.dma(gb, pbc_d[l][:, 0:512].partition_broadcast(128), "ggb", w=["ggb"])
            MS(vpf, 0.0, w=["gvp"])
            for h in range(4):
                TT(WTm[:, h, :], WTr[:, h, :], maskle[:], ALU.mult, r=["gWTr"], w=["gWTm"])
            for c in range(2):
                for q in range(4):
                    CP(bs4[:, c, q, :], bsT[:, c, :], r=["gbsT"], w=["gbs4"])
            for g in range(4):
                sl = slice(512 * g, 512 * g + 512)
                for c in range(2):
                    pu = psn()
                    for dc in range(8):
                        MM(pu.ap, wv3[:, dc, 128 * c:128 * c + 128], HT[:, dc, sl], dc == 0, dc == 7, r=slot.names + ["HT%d" % g], w=[pu.name])
                    ACT(uT[:, c, sl], pu.ap, AF.Gelu, r=[pu.name], w=["guT%d" % g])
            mv = small[:, 420:452].rearrange("p (t k) -> p t k", k=2)
            lnb = small[:, 452:468]
            rsb = small[:, 468:484]
            for t in range(NT):
                g = t // 4
                pv = psn()
                for dc in range(8):
                    MM(pv.ap[:, 0:256], HT[:, dc, 128 * t:128 * t + 128], wv3[:, dc, 256:512], dc == 0, dc == 7, r=slot.names + ["HT%d" % g], w=[pv.name])
                gv = gvb[t % 2]
                gvn = "ggv%d" % (t % 2)
                ACT(gv, pv.ap[:, 0:256], AF.Gelu, r=[pv.name], w=[gvn])
                P.op("dve", lambda e, gv=gv: e.bn_stats(out=bnst[:, 0:6], in_=gv), r=[gvn], w=["gbn6"])
                P.op("dve", lambda e, t=t: e.bn_aggr(out=mv[:, t, :], in_=bnst[:, 0:6]), r=["gbn6"], w=["gmv"])
                gv3 = gv.rearrange("p (h d) -> p h d", d=64)
                for hh in range(2):
                    CP(vp[:, t, hh::2, 64 * hh:64 * hh + 64], gv3[:, hh::2, :], r=[gvn, "gvp"], w=["gvp%d" % t])
            ACT(lnb, mv[:, :, 1], AF.Ln, r=["gmv"], w=["glnb"], bias=EPS)
            ACT(rsb, lnb, AF.Exp, r=["glnb"], w=["grsb"], scale=-0.5)
            g3 = gb[:, 0:256].rearrange("p (h d) -> p h d", d=64)
            b3 = gb[:, 256:512].rearrange("p (h d) -> p h d", d=64)
            for t in range(NT):
                for hh in range(2):
                    v_ = vp[:, t, hh::2, 64 * hh:64 * hh + 64]
                    TS(v_, v_, mv[:, t, 0:1], rsb[:, t:t + 1], ALU.subtract, ALU.mult, r=["gvp%d" % t, "gmv", "grsb"], w=["gvp%d" % t])
                    TT(v_, v_, g3[:, hh::2, :], ALU.mult, r=["gvp%d" % t, "ggb"], w=["gvp%d" % t])
                    TT(v_, v_, b3[:, hh::2, :], ALU.add, r=["gvp%d" % t, "ggb"], w=["gvp%d" % t])
            for g in range(4):
                sl = slice(512 * g, 512 * g + 512)
                for c in range(2):
                    pm = psn()
                    for q in range(4):
                        cb = 4 * g + q
                        for hh in range(2):
                            h = 2 * c + hh
                            MM(pm.ap[:, 128 * q:128 * q + 128], vp[:, cb, h, :], WTm[:, h, :], hh == 0, hh == 1, r=["gvp%d" % cb, "gWTm"], w=[pm.name])
                    TT(tmp, pm.ap, bs4[:, c, :, :].rearrange("p q t -> p (q t)"), ALU.add, r=[pm.name, "gbs4"], w=["gtmp"])
                    TT(YT[:, c, sl], tmp, uT[:, c, sl], ALU.mult, r=["gtmp", "guT%d" % g], w=["YT%d_%d" % (c, g)])
            group_norm_out(l, 2)

        def phase_lru(l):
            slot = ws_next()
            wv3 = slot.ap[:, 0:4096].rearrange("p (c n) -> p c n", c=8)
            P.dma(wv3, w_in_view(l)[:, :, 1792:2304], slot.name, w=slot.names, eng="pool")
            yield
            A.reset()
            xpad = A.f32(2052)
            xb = A.f32(2048)
            xbb = A.bf16(2048)
            gl = A.bf16(2048)
            bdf = A.bf16(512)
            bd = bdf.rearrange("p (a c n) -> p a c n", a=2, c=2)
            hq = [A.f32(512) for _ in range(2)]
            r_ = A.f32(512)
            i_ = A.f32(512)
            a_ = A.f32(512)
            s_ = A.f32(512)
            b_ = A.f32(512)
            MS(bdf, 0.0, w=["lbd"])
            for a, src in enumerate([wa_d, wx_d]):
                for c in range(2):
                    for hh in range(2):
                        P.dma(bd[64 * hh:64 * hh + 64, a, c, 64 * hh:64 * hh + 64], src[l, 2 * c + hh], "lbd", r=[], w=["lbd"], eng="pool")
            ACT(clc[:, 4:6], pc(l, 36, 2), AF.Exp, r=[], w=["lcl_t"], scale=-1.0)
            ACT(clc[:, 6:8], clc[:, 4:6], AF.Ln, r=["lcl_t"], w=["lcl_u"], bias=1.0)
            TS(clc[:, 0:2], clc[:, 6:8], -8.0, None, ALU.mult, None, r=["lcl_u"], w=["lcl"])
            TS(clc[:, 2:4], clc[:, 6:8], -16.0, None, ALU.mult, None, r=["lcl_u"], w=["lcl"])
            for c in range(2):
                MS(xpad[:, 0:3], 0.0, w=["lxp"])
                for g in range(4):
                    sl = slice(512 * g, 512 * g + 512)
                    px = psn()
                    pg = psn()
                    for dc in range(8):
                        MM(px.ap, wv3[:, dc, 128 * c:128 * c + 128], HT[:, dc, sl], dc == 0, dc == 7, r=slot.names + ["HT%d" % g], w=[px.name])
                    for dc in range(8):
                        MM(pg.ap, wv3[:, dc, 256 + 128 * c:256 + 128 * c + 128], HT[:, dc, sl], dc == 0, dc == 7, r=slot.names + ["HT%d" % g], w=[pg.name])
                    ACT(xpad[:, 3 + 512 * g:3 + 512 * g + 512], px.ap, AF.Copy, r=[px.name], w=["lxp"])
                    ACT(gl[:, sl], pg.ap, AF.Gelu, r=[pg.name], w=["lgl"])
                TS(xb, xpad[:, 0:2048], pc(l, 102 + 4 * c), pc(l, 30 + c), ALU.mult, ALU.add, r=["lxp"], w=["lxb"])
                for k in range(1, 4):
                    STT(xb, xpad[:, k:k + 2048], pc(l, 102 + 4 * c + k), xb, ALU.mult, ALU.add, r=["lxp", "lxb"], w=["lxb"])
                ACT(xbb, xb, AF.Copy, r=["lxb"], w=["lxbb"])
                for g in range(4):
                    sl = slice(512 * g, 512 * g + 512)
                    pr_ = psn()
                    pi_ = psn()
                    MM(pr_.ap, bd[:, 0, c, :], xbb[:, sl], True, True, r=["lbd", "lxbb"], w=[pr_.name])
                    MM(pi_.ap, bd[:, 1, c, :], xbb[:, sl], True, True, r=["lbd", "lxbb"], w=[pi_.name])
                    ACT(r_, pr_.ap, AF.Sigmoid, r=[pr_.name], w=["lr"], bias=pc(l, 32 + c))
                    ACT(i_, pi_.ap, AF.Sigmoid, r=[pi_.name], w=["li"], bias=pc(l, 34 + c))
                    ACT(a_, r_, AF.Exp, r=["lr", "lcl"], w=["la"], scale=clc[:, c:c + 1])
                    ACT(s_, r_, AF.Exp, r=["lr", "lcl"], w=["ls"], scale=clc[:, 2 + c:3 + c])
                    ACT(s_, s_, AF.Sqrt, r=["ls"], w=["ls"], scale=-1.0, bias=1.0)
                    TT(b_, i_, xb[:, sl], ALU.mult, r=["li", "lxb"], w=["lb"])
                    TT(b_, b_, s_, ALU.mult, r=["lb", "ls"], w=["lb"])
                    hcur = hq[g % 2]
                    hprev = hq[(g + 1) % 2]
                    init = 0.0 if g == 0 else hprev[:, 511:512]
                    rd = ["la", "lb"] + ([] if g == 0 else ["lh%d" % ((g + 1) % 2)])
                    P.op("dve", lambda e, hcur=hcur, init=init: e.tensor_tensor_scan(out=hcur, data0=a_, data1=b_, initial=init, op0=ALU.mult, op1=ALU.add),
                         r=rd, w=["lh%d" % (g % 2)])
                    TT(YT[:, c, sl], hcur, gl[:, sl], ALU.mult, r=["lh%d" % (g % 2), "lgl"], w=["YT%d_%d" % (c, g)])
            group_norm_out(l, 3)

        def phase_router(l):
            yield
            A.reset()
            rfm = A.f32(64).rearrange("p (c e) -> p c e", c=8)
            Rg = A.f32(64).rearrange("p (c e) -> p c e", c=8)
            hn = [A.f32(1024) for _ in range(2)]
            hT = [A.f32(1024) for _ in range(2)]
            P.dma(rfm, routerF_d.rearrange("p (c e) -> p c e", c=8), "rtB", w=["rfm"])
            for dc in range(8):
                TS(Rg[:, dc, :], rfm[:, dc, :], pc(l, 8 + dc), None, ALU.mult, None, r=["rfm"], w=["Rg"])
            for t in range(NT):
                h_ = hn[t % 2]
                hnn = "rhn%d" % (t % 2)
                hT_ = hT[t % 2]
                TS(h_, X[:, t, :], rs[:, t:t + 1], None, ALU.mult, None, r=["X%d" % t, "rs"], w=[hnn])
                for half in range(2):
                    pb = psn()
                    for i in range(4):
                        dc = 4 * half + i
                        P.op("pe", lambda e, o=pb.ap[:, 128 * i:128 * i + 128], a=h_[:, 128 * dc:128 * dc + 128]:
                             e.transpose(out=o, in_=a, identity=identf[:]), r=[hnn], w=[pb.name])
                    htn = "rhT%d_%d" % (t % 2, half)
                    if half == 0:
                        ACT(hT_[:, 0:512], pb.ap, AF.Copy, r=[pb.name], w=[htn])
                    else:
                        CP(hT_[:, 512:1024], pb.ap, r=[pb.name], w=[htn])
                pl = psn()
                for dc in range(8):
                    MM(pl.ap[:, 0:8], hT_[:, 128 * dc:128 * dc + 128], Rg[:, dc, :], dc == 0, dc == 7,
                       r=["rhT%d_%d" % (t % 2, dc // 4), "Rg"], w=[pl.name])
                CP(logits[:, t, :], pl.ap[:, 0:8], r=[pl.name], w=["rlg%d" % t])
                lg = logits[:, t, :]
                m1 = tk[:, 0:1]
                m2 = tk[:, 1:2]
                dd = tk[:, 2:3]
                g1 = tk[:, 3:4]
                g2 = tk[:, 4:5]
                eq1 = tk[:, 8:16]
                eq2 = tk[:, 16:24]
                l2 = tk[:, 24:32]
                P.op("dve", lambda e, lg=lg: e.reduce_max(out=m1, in_=lg, axis=AX.X), r=["rlg%d" % t], w=["rm1"])
                TS(eq1, lg, m1, None, ALU.is_equal, None, r=["rlg%d" % t, "rm1"], w=["req1"])
                STT(l2, eq1, -1e30, lg, ALU.mult, ALU.add, r=["req1", "rlg%d" % t], w=["rl2"])
                P.op("dve", lambda e: e.reduce_max(out=m2, in_=l2, axis=AX.X), r=["rl2"], w=["rm2"])
                TS(eq2, l2, m2, None, ALU.is_equal, None, r=["rl2", "rm2"], w=["req2"])
                TT(dd, m1, m2, ALU.subtract, r=["rm1", "rm2"], w=["rdd"])
                ACT(g1, dd, AF.Sigmoid, r=["rdd"], w=["rg1"])
                ACT(g2, dd, AF.Sigmoid, r=["rdd"], w=["rgg2"], scale=-1.0)
                TS(gates[:, t, :], eq1, g1, None, ALU.mult, None, r=["req1", "rg1"], w=["gates"])
                STT(gates[:, t, :], eq2, g2, gates[:, t, :], ALU.mult, ALU.add, r=["req2", "rgg2", "gates"], w=["gates"])

        def phase_ffn(l, moe):
            if moe:
                groups = [(e, f) for e in range(NE) for f in range(DFE // 256)]
            else:
                groups = [(0, f) for f in range(DFF // 256)]

            def load_up(n, slot):
                e, f = groups[n]
                gsrc = mg_d[0, e] if moe else fg_d[0]
                usrc = mu_d[0, e] if moe else fu_d[0]
                for j, src in enumerate([gsrc, usrc]):
                    P.dma(slot.ap[:, 2048 * j:2048 * j + 2048].rearrange("p (c n) -> p c n", c=8),
                          src.rearrange("(c p) n -> p c n", p=128)[:, :, 256 * f:256 * f + 256],
                          slot.names[0], w=[slot.names[0]], eng="pool")

            def load_dn(n, slot):
                e, f = groups[n]
                dsrc = md_d[0, e] if moe else fd_d[0]
                P.dma(slot.ap[:, 4096:6144].rearrange("p (c n) -> p c n", c=2),
                      dsrc[256 * f:256 * f + 256, :].rearrange("(c p) n -> p c n", p=128),
                      slot.names[1], w=[slot.names[1]], eng="pool")

            base = cnt["ws"]
            cnt["ws"] += len(groups)

            def slot_of(n):
                return ws_slot((base + n) % 2)

            s0 = slot_of(0)
            load_up(0, s0)
            load_dn(0, s0)
            yield
            A.reset()
            AT = [A.bf16(4096).rearrange("p (c n) -> p c n", c=2) for _ in range(2)]
            sgb = [A.f32(512) for _ in range(2)]
            if len(groups) > 1:
                s1 = slot_of(1)
                load_up(1, s1)
                load_dn(1, s1)
            k = [0]

            def up(n):
                slot = slot_of(n)
                at = AT[n % 2]
                wg3 = slot.ap[:, 0:2048].rearrange("p (c n) -> p c n", c=8)
                wu3 = slot.ap[:, 2048:4096].rearrange("p (c n) -> p c n", c=8)
                for g in range(4):
                    sl = slice(512 * g, 512 * g + 512)
                    for fc in range(2):
                        pg = psn()
                        pu = psn()
                        for dc in range(8):
                            MM(pg.ap, wg3[:, dc, 128 * fc:128 * fc + 128], HT[:, dc, sl], dc == 0, dc == 7, r=[slot.names[0], "HT%d" % g], w=[pg.name])
                        for dc in range(8):
                            MM(pu.ap, wu3[:, dc, 128 * fc:128 * fc + 128], HT[:, dc, sl], dc == 0, dc == 7, r=[slot.names[0], "HT%d" % g], w=[pu.name])
                        sg = sgb[k[0] % 2]
                        sgn = "fsg%d" % (k[0] % 2)
                        k[0] += 1
                        ACT(sg, pg.ap, AF.Silu, r=[pg.name], w=[sgn])
                        TT(at[:, fc, sl], sg, pu.ap, ALU.mult, r=[sgn, pu.name], w=["fAT%d" % (n % 2)])
                        yield

            def down(n):
                e, f = groups[n]
                slot = slot_of(n)
                at = AT[n % 2]
                wd3 = slot.ap[:, 4096:6144].rearrange("p (c n) -> p c n", c=2)
                for t in range(NT):
                    for h in range(2):
                        py = psn()
                        for fc in range(2):
                            MM(py.ap, at[:, fc, 128 * t:128 * t + 128], wd3[:, fc, 512 * h:512 * h + 512], fc == 0, fc == 1,
                               r=["fAT%d" % (n % 2), slot.names[1]], w=[py.name])
                        xs_ = X[:, t, 512 * h:512 * h + 512]
                        if moe:
                            STT(xs_, py.ap, gates[:, t, e:e + 1], xs_, ALU.mult, ALU.add, r=[py.name, "X%d" % t, "gates"], w=["X%d" % t])
                        else:
                            TT(xs_, py.ap, xs_, ALU.add, r=[py.name, "X%d" % t], w=["X%d" % t])
                        yield

            N = len(groups)
            for _ in up(0):
                pass
            for n in range(N):
                ug = up(n + 1) if n + 1 < N else None
                dg = down(n)
                for step in range(8):
                    if ug is not None:
                        next(ug, None)
                    for _ in range(4):
                        next(dg, None)
                if ug is not None:
                    for _ in ug:
                        pass
                for _ in dg:
                    pass
                if n + 2 < N:
                    load_up(n + 2, slot_of(n + 2))
                    load_dn(n + 2, slot_of(n + 2))

        gens = []
        for sq in range(nseq):
            gens.append(phase_load(sq))
            for l in layers:
                gens.append(phase_norm(l, 0))
                gens.append(phase_attn(l, 0))
                gens.append(phase_attn(l, 1))
                gens.append(phase_conf(l))
                gens.append(phase_gmlp(l))
                gens.append(phase_lru(l))
                gens.append(phase_norm(l, 8))
                if l % 2 == 1:
                    gens.append(phase_router(l))
                gens.append(phase_ffn(l, l % 2 == 1))
            gens.append(phase_store(sq))
        if n_phases is not None:
            gens = gens[:n_phases] + [phase_store(0)]
        next(gens[0])
        for i, g in enumerate(gens):
            for _ in g:
                pass
            if i + 1 < len(gens):
                next(gens[i + 1])
            P.fence()
        P.wait_all("sp", stores)
        P.emit(st)
    return nc


def prep_shared(inp):
    f = lambda a: np.ascontiguousarray(np.asarray(a, dtype=np.float32))
    pcol = np.zeros((128, L * NPC), np.float32)

    def col(v, n):
        return np.asarray(v, np.float32).reshape(n, 128).T

    for l in range(L):
        b = l * NPC
        pcol[:, b + 0:b + 8] = col(inp["norm1_g"][l], 8)
        pcol[:, b + 8:b + 16] = col(inp["norm2_g"][l], 8)
        pcol[:, b + 16:b + 24] = col(inp["group_norm_g"][l], 8)
        pcol[:, b + 24:b + 26] = col(inp["conf_dw_b"][l], 2)
        pcol[:, b + 26:b + 28] = col(inp["conf_ln_g"][l], 2)
        pcol[:, b + 28:b + 30] = col(inp["conf_ln_b"][l], 2)
        pcol[:, b + 30:b + 32] = col(inp["lru_conv_b"][l], 2)
        pcol[:, b + 32:b + 34] = col(inp["lru_ba"][l], 2)
        pcol[:, b + 34:b + 36] = col(inp["lru_bx"][l], 2)
        pcol[:, b + 36:b + 38] = col(inp["lru_lambda"][l], 2)
        pcol[:, b + 38] = np.tile(np.asarray(inp["q_norm_g"][l], np.float32), 2)
        pcol[:, b + 39] = np.tile(np.asarray(inp["k_norm_g"][l], np.float32), 2)
        cw = np.asarray(inp["conf_dw_w"][l], np.float32)
        lw = np.asarray(inp["lru_conv_w"][l], np.float32)
        for c in range(2):
            pcol[:, b + 40 + 31 * c:b + 40 + 31 * c + 31] = cw[:, 128 * c:128 * c + 128].T
            pcol[:, b + 102 + 4 * c:b + 102 + 4 * c + 4] = lw[:, 128 * c:128 * c + 128].T
    pbc = np.zeros((L, 1, 1536), np.float32)
    wsT = np.zeros((L, 128, 512), np.float32)
    bsT = np.zeros((L, 128, 256), np.float32)
    for l in range(L):
        pbc[l, 0, 0:256] = inp["gmlp_ln_g"][l]
        pbc[l, 0, 256:512] = inp["gmlp_ln_b"][l]
        pbc[l, 0, 512:1536] = inp["norm2_g"][l]
        wsT[l] = np.asarray(inp["gmlp_ws"][l], np.float32).transpose(2, 0, 1).reshape(128, 512)
        bsT[l] = np.repeat(np.asarray(inp["gmlp_bs"][l], np.float32), 64, axis=0).reshape(2, 128, 128).transpose(1, 0, 2).reshape(128, 256)
    routerF = np.ascontiguousarray(np.asarray(inp["moe_router"][0], np.float32).reshape(8, 128, NE).transpose(1, 0, 2)).reshape(128, 8 * NE)
    return {
        "w_in": f(inp["w_in"]), "w_out": f(inp["w_out"]),
        "ffn_w_gate": f(inp["ffn_w_gate"]), "ffn_w_up": f(inp["ffn_w_up"]), "ffn_w_down": f(inp["ffn_w_down"]),
        "moe_w_gate": f(inp["moe_w_gate"]), "moe_w_up": f(inp["moe_w_up"]), "moe_w_down": f(inp["moe_w_down"]),
        "pcol": pcol, "pbc": pbc, "wsT": wsT, "bsT": bsT, "routerF": routerF,
        "lru_wa": f(inp["lru_wa"]), "lru_wx": f(inp["lru_wx"]),
    }


def kernel(**inputs):
    x = np.ascontiguousarray(np.asarray(inputs["x"], dtype=np.float32))
    B = x.shape[0]
    nseq = B // N_CORES
    shared = prep_shared(inputs)
    nc = build_nc(nseq=nseq)
    in_maps = []
    for c in range(N_CORES):
        m = dict(shared)
        m["x"] = x[c * nseq:(c + 1) * nseq]
        in_maps.append(m)
    res = run_bass_kernel_spmd(nc, in_maps, core_ids=list(range(N_CORES)))
    out = np.concatenate([np.asarray(r["y"], dtype=np.float32) for r in res.results], axis=0)
    return out
```

```python
import numpy as np
from contextlib import ExitStack
import concourse.bass as bass
import concourse.mybir as mybir
from concourse.bass_utils import run_bass_kernel_spmd

F32 = mybir.dt.float32
BF16 = mybir.dt.bfloat16
I32 = mybir.dt.int32
F32R = mybir.dt.float32r
AF = mybir.ActivationFunctionType
ALU = mybir.AluOpType
AX = mybir.AxisListType

D = 1024
S = 2048
NT = 16
L = 2
INC = 2304
DFF = 2816
NE = 8
DFE = 3584
EPS = 1e-6
NPC = 110
N_CORES = 8


class Prog:
    def __init__(self, nc):
        self.nc = nc
        self.ops = []
        self.last_w = {}
        self.readers = {}
        self.dma_cnt = {}
        self.dma_last = {}
        self.dma_arena = {}
        self.fence_deps = {}
        self.fence_pending = set()

    def _deps(self, r, w):
        deps = {}
        for x in r:
            lw = self.last_w.get(x)
            if lw is not None:
                deps[lw] = True
        for x in w:
            lw = self.last_w.get(x)
            if lw is not None:
                deps.setdefault(lw, False)
            for rd in self.readers.get(x, ()):
                deps.setdefault(rd, False)
        i = len(self.ops)
        for x in r:
            self.readers.setdefault(x, []).append(i)
        for x in w:
            self.last_w[x] = i
            self.readers[x] = []
        return deps

    @staticmethod
    def _persistent(name):
        return name.startswith(("X", "HT", "YT", "WS", "ps"))

    def _fence_check(self, eng, r, w, deps):
        arena = any(not self._persistent(x) for x in r) or any(not self._persistent(x) for x in w)
        if arena and eng in self.fence_pending:
            self.fence_pending.discard(eng)
            for i in self.fence_deps:
                deps[i] = True
        return arena

    def fence(self):
        last = {}
        for i, o in enumerate(self.ops):
            if o["dma"] is None and o["fn"] is not None:
                last[o["eng"]] = i
        deps = {i: True for i in last.values()}
        for k, i in self.dma_last.items():
            if self.dma_arena.get(k):
                deps[i] = True
        self.fence_deps = deps
        self.fence_pending = set(["pe", "act", "dve", "pool", "sp"])

    def op(self, eng, fn, r=(), w=()):
        deps = self._deps(r, w)
        self._fence_check(eng, r, w, deps)
        self.ops.append(dict(eng=eng, fn=fn, deps=deps, dma=None))
        return len(self.ops) - 1

    def dma(self, out, in_, key, r=(), w=(), eng="sp"):
        deps = self._deps(r, w)
        self.dma_arena[key] = self._fence_check(eng, r, w, deps)
        self.dma_cnt[key] = self.dma_cnt.get(key, 0) + 16
        fn = lambda e: e.dma_start(out=out, in_=in_)
        self.ops.append(dict(eng=eng, fn=fn, deps=deps, dma=(key, self.dma_cnt[key])))
        self.dma_last[key] = len(self.ops) - 1
        return len(self.ops) - 1

    def wait_all(self, eng, idxs):
        self.ops.append(dict(eng=eng, fn=None, deps={i: True for i in idxs}, dma=None))

    def barrier(self):
        last = {}
        for i, o in enumerate(self.ops):
            if o["dma"] is None and o["fn"] is not None:
                last[o["eng"]] = i
        deps = {i: True for i in last.values()}
        for i in self.dma_last.values():
            deps[i] = True
        for eng in ("pe", "act", "dve", "pool", "sp"):
            self.ops.append(dict(eng=eng, fn=None, deps=dict(deps), dma=None, bar=True))
        self.fence_deps = {}
        self.fence_pending = set()

    def emit(self, stack):
        nc = self.nc
        ops = self.ops
        for o in ops:
            o["sig"] = False
        for o in ops:
            latest = {}
            for j, raw in o["deps"].items():
                p = ops[j]
                if p["dma"] is not None:
                    continue
                if p["eng"] == o["eng"]:
                    if o["eng"] == "pe" or o.get("bar"):
                        continue
                if j > latest.get(p["eng"], -1):
                    latest[p["eng"]] = j
            o["edeps"] = latest
            for j in latest.values():
                ops[j]["sig"] = True
        cnt = {}
        for o in ops:
            if o["dma"] is None and o["sig"]:
                cnt[o["eng"]] = cnt.get(o["eng"], 0) + 1
                o["sigval"] = cnt[o["eng"]]
        engs = ["pe", "act", "dve", "pool", "sp"]
        sems = {e: stack.enter_context(nc.semaphore("s_" + e)) for e in engs}
        dsems = {k: stack.enter_context(nc.semaphore("d_%d" % n)) for n, k in enumerate(self.dma_cnt)}
        block = stack.enter_context(nc.Block())

        def run(ename, eng):
            waited = {}
            for o in ops:
                if o["eng"] != ename:
                    continue
                wl = []
                for j, raw in o["deps"].items():
                    p = ops[j]
                    if p["dma"] is not None:
                        key = p["dma"][0]
                        wl.append((dsems[key], p["dma"][1], "d" + str(key)))
                for pe_, j in o["edeps"].items():
                    wl.append((sems[pe_], ops[j]["sigval"], pe_))
                for s, v, wk in wl:
                    if waited.get(wk, 0) >= v:
                        continue
                    waited[wk] = v
                    eng.wait_ge(s, v)
                if o["fn"] is None:
                    continue
                ins = o["fn"](eng)
                if o["dma"] is not None:
                    ins.then_inc(dsems[o["dma"][0]], 16)
                elif o["sig"]:
                    ins.then_inc(sems[ename], 1)

        @block.tensor
        def _(e):
            run("pe", e)

        @block.scalar
        def _(e):
            run("act", e)

        @block.vector
        def _(e):
            run("dve", e)

        @block.gpsimd
        def _(e):
            run("pool", e)

        @block.sync
        def _(e):
            run("sp", e)


class Res:
    def __init__(self, ap, name, names=None):
        self.ap = ap
        self.name = name
        self.names = names if names is not None else [name]


def build_nc(nseq=2, n_phases=None, layers=(0, 1)):
    nc = bass.Bass("TRN2", target_bir_lowering=False)

    def din(name, shape):
        return nc.dram_tensor(name, list(shape), F32, kind="ExternalInput").ap()

    x_d = din("x", [nseq, S, D])
    w_in_d = din("w_in", [L, D, INC])
    w_out_d = din("w_out", [L, D, D])
    fg_d = din("ffn_w_gate", [1, D, DFF])
    fu_d = din("ffn_w_up", [1, D, DFF])
    fd_d = din("ffn_w_down", [1, DFF, D])
    mg_d = din("moe_w_gate", [1, NE, D, DFE])
    mu_d = din("moe_w_up", [1, NE, D, DFE])
    md_d = din("moe_w_down", [1, NE, DFE, D])
    pcol_d = din("pcol", [128, L * NPC])
    pbc_d = din("pbc", [L, 1, 1536])
    wsT_d = din("wsT", [L, 128, 512])
    bsT_d = din("bsT", [L, 128, 256])
    routerF_d = din("routerF", [128, 8 * NE])
    wa_d = din("lru_wa", [L, 4, 64, 64])
    wx_d = din("lru_wx", [L, 4, 64, 64])
    y_d = nc.dram_tensor("y", [nseq, S, D], F32, kind="ExternalOutput").ap()

    with ExitStack() as st:
        P = Prog(nc)

        def sb(name, shape, dt=F32):
            return st.enter_context(nc.sbuf_tensor(name, shape, dt))

        X = sb("X", [128, NT, D])
        HT = sb("HT", [128, 8, S], BF16)
        YT = sb("YT", [128, 2, S], BF16)
        WS = [sb("WS%d" % i, [128, 6144], BF16) for i in range(2)]
        SCR = sb("SCR", [128, 12288])
        GNS = SCR[:, 10240:12288]
        SPR = sb("SPR", [128, 10 * 512], F32R)
        identb = sb("identb", [128, 128], BF16)
        identf = sb("identf", [128, 128])
        onesf = sb("onesf", [128, 128])
        onesb = sb("onesb", [128, 128], BF16)
        blkonesb = sb("blkonesb", [128, 128], BF16)
        ntri = sb("ntri", [128, 128], F32R)
        nones = sb("nones", [128, 128], F32R)
        MW = sb("MW", [128, 896])
        NEGW = sb("NEGW", [128, 896], BF16)
        maskle = sb("maskle", [128, 128])
        pcol = sb("pcol_s", [128, L * NPC])
        small = sb("small", [128, 512])
        PS = [st.enter_context(nc.psum_tensor("ps%d" % i, [128, 512], F32)) for i in range(8)]

        ss = small[:, 0:16]
        lnt = small[:, 16:32]
        rs = small[:, 32:48]
        qk8 = small[:, 48:52]
        clc = small[:, 52:60]
        logits = small[:, 64:192].rearrange("p (t e) -> p t e", e=NE)
        gates = small[:, 192:320].rearrange("p (t e) -> p t e", e=NE)
        tk = small[:, 320:400]
        bnst = small[:, 400:420]

        cnt = {"ps": 0, "psl": 0, "ws": 0}

        def psn():
            i = cnt["ps"] % 6
            cnt["ps"] += 1
            return Res(PS[i][:], "ps%d" % i)

        def psl():
            i = 6 + cnt["psl"] % 2
            cnt["psl"] += 1
            return Res(PS[i][:], "ps%d" % i)

        def ws_slot(i):
            return Res(WS[i][:], "WS%du" % i, ["WS%du" % i, "WS%dd" % i])

        def ws_next():
            i = cnt["ws"] % 2
            cnt["ws"] += 1
            return ws_slot(i)

        class Arena:
            def __init__(self, ap):
                self.ap = ap
                self.off = 0

            def reset(self):
                self.off = 0

            def f32(self, n):
                a = self.ap[:, self.off:self.off + n]
                self.off += n
                assert self.off <= self.ap.shape[1], self.off
                return a

            def bf16(self, n):
                assert n % 2 == 0
                return self.f32(n // 2).bitcast(BF16)

        A = Arena(SCR[:])

        def MM(out, lhsT, rhs, start, stop, r, w):
            P.op("pe", lambda e: e.matmul(out, lhsT=lhsT, rhs=rhs, start=start, stop=stop), r=r, w=w)

        def TR(out, in_, r, w):
            P.op("pe", lambda e: e.transpose(out=out, in_=in_, identity=identb[:]), r=r, w=w)

        def ACT(out, in_, func, r, w, **kw):
            P.op("act", lambda e: e.activation(out=out, in_=in_, func=func, **kw), r=r, w=w)

        def TT(out, in0, in1, op, r, w, eng="dve"):
            P.op(eng, lambda e: e.tensor_tensor(out=out, in0=in0, in1=in1, op=op), r=r, w=w)

        def TS(out, in0, s1, s2, op0, op1, r, w, eng="dve"):
            if s2 is None:
                P.op(eng, lambda e: e.tensor_scalar(out=out, in0=in0, scalar1=s1, scalar2=None, op0=op0), r=r, w=w)
            else:
                P.op(eng, lambda e: e.tensor_scalar(out=out, in0=in0, scalar1=s1, scalar2=s2, op0=op0, op1=op1), r=r, w=w)

        def STT(out, in0, scalar, in1, op0, op1, r, w, **kw):
            P.op("dve", lambda e: e.scalar_tensor_tensor(out=out, in0=in0, scalar=scalar, in1=in1, op0=op0, op1=op1, **kw), r=r, w=w)

        def CP(out, in_, r, w, eng="dve"):
            P.op(eng, lambda e: e.tensor_copy(out=out, in_=in_), r=r, w=w)

        def MS(ap, val, w, eng="dve"):
            P.op(eng, lambda e: e.memset(ap, val), w=w)

        def pc(l, col, n=1):
            return pcol[:, l * NPC + col: l * NPC + col + n]

        io = SCR[:, 0:896].bitcast(I32)
        iof = SCR[:, 896:1792]
        P.op("pool", lambda e: e.iota(io, pattern=[[1, 896]], base=0, channel_multiplier=-1), w=["io"])
        CP(iof, io, r=["io"], w=["iof"])
        P.op("dve", lambda e: e.tensor_single_scalar(out=MW[:], in_=iof, scalar=384.0, op=ALU.is_gt), r=["iof"], w=["cMW"])
        P.op("dve", lambda e: e.tensor_scalar(out=NEGW[:], in0=MW[:], scalar1=30000.0, scalar2=-30000.0, op0=ALU.mult, op1=ALU.add), r=["cMW"], w=["c"])
        P.op("dve", lambda e: e.tensor_single_scalar(out=identf[:], in_=iof[:, 0:128], scalar=0.0, op=ALU.is_equal), r=["iof"], w=["c"])
        P.op("dve", lambda e: e.tensor_single_scalar(out=identb[:], in_=iof[:, 0:128], scalar=0.0, op=ALU.is_equal), r=["iof"], w=["c"])
        P.op("dve", lambda e: e.tensor_single_scalar(out=maskle[:], in_=iof[:, 0:128], scalar=0.0, op=ALU.is_ge), r=["iof"], w=["c"])
        P.op("dve", lambda e: e.tensor_scalar(out=ntri[:], in0=iof[:, 0:128], scalar1=0.0, scalar2=-1.0, op0=ALU.is_le, op1=ALU.mult), r=["iof"], w=["c"])
        MS(onesf[:], 1.0, w=["c"])
        MS(onesb[:], 1.0, w=["c"])
        P.op("dve", lambda e: e.tensor_scalar(out=nones[:], in0=iof[:, 0:128], scalar1=0.0, scalar2=-1.0, op0=ALU.mult, op1=ALU.add), r=["iof"], w=["c"])
        MS(blkonesb[:], 0.0, w=["c"])
        MS(blkonesb[0:64, 0:64], 1.0, w=["c"])
        MS(blkonesb[64:128, 64:128], 1.0, w=["c"])
        P.dma(pcol[:], pcol_d, "pcol", w=["pcol"])
        for l in range(L):
            TS(qk8[:, l:l + 1], pc(l, 38), 0.125, None, ALU.mult, None, r=["pcol"], w=["c"])
        P.barrier()

        def phase_load(sq):
            yield
            xs = x_d[sq].rearrange("(t p) d -> p t d", p=128)
            for g in range(4):
                P.dma(X[:, 4 * g:4 * g + 4, :], xs[:, 4 * g:4 * g + 4, :], "xin%d" % g, w=["X%d" % t for t in range(4 * g, 4 * g + 4)])

        stores = []

        def phase_store(sq):
            yield
            ys = y_d[sq].rearrange("(t p) d -> p t d", p=128)
            for g in range(4):
                stores.append(P.dma(ys[:, 4 * g:4 * g + 4, :], X[:, 4 * g:4 * g + 4, :], "xout%d" % g, r=["X%d" % t for t in range(4 * g, 4 * g + 4)]))

        def phase_norm(l, gcol0):
            yield
            A.reset()
            hss = [A.bf16(4096).rearrange("p (i d) -> p i d", i=4) for _ in range(2)]
            junk = A.bf16(1024)

            def N1(g):
                hs = hss[g % 2]
                for i in range(4):
                    t = 4 * g + i
                    ACT(junk, X[:, t, :], AF.Square, r=["X%d" % t], w=["junk", "ss"], accum_out=ss[:, t:t + 1])
                ACT(lnt[:, 4 * g:4 * g + 4], ss[:, 4 * g:4 * g + 4], AF.Ln, r=["ss"], w=["lnt"], scale=1.0 / D, bias=EPS)
                ACT(rs[:, 4 * g:4 * g + 4], lnt[:, 4 * g:4 * g + 4], AF.Exp, r=["lnt"], w=["rs"], scale=-0.5)
                for i in range(4):
                    t = 4 * g + i
                    TS(hs[:, i, :], X[:, t, :], rs[:, t:t + 1], None, ALU.mult, None, r=["X%d" % t, "rs"], w=["hs%d_%d" % (g % 2, i)])

            def N2(g):
                hs = hss[g % 2]
                for c in range(8):
                    pb = psn()
                    ptb = pb.ap.bitcast(BF16)
                    for i in range(4):
                        TR(ptb[:, 128 * i:128 * i + 128], hs[:, i, 128 * c:128 * c + 128], r=["hs%d_%d" % (g % 2, i)], w=[pb.name])
                    dst = HT[:, c, 512 * g:512 * g + 512]
                    if c % 2 == 0:
                        ACT(dst, ptb[:, 0:512], AF.Identity, r=[pb.name], w=["HT%d" % g], scale=pc(l, gcol0 + c))
                    else:
                        TS(dst, ptb[:, 0:512], pc(l, gcol0 + c), None, ALU.mult, None, r=[pb.name], w=["HT%d" % g])

            N1(0)
            for g in range(4):
                if g + 1 < 4:
                    N1(g + 1)
                N2(g)

        def w_in_view(l):
            return w_in_d[l].rearrange("(c p) n -> p c n", p=128)

        def group_norm_out(l, gi):
            slot = ws_next()
            w3 = slot.ap[:, 0:2048].rearrange("p (c n) -> p c n", c=2)
            P.dma(w3, w_out_d[l][256 * gi:256 * gi + 256, :].rearrange("(c p) n -> p c n", p=128), slot.name,
                  w=slot.names, eng="pool")
            sqb = [GNS[:, 256 * i:256 * i + 256].bitcast(BF16) for i in range(4)]
            lrs = [GNS[:, 1024:1536], GNS[:, 1536:2048]]
            gheld = {}

            def GP(g):
                sl = slice(512 * g, 512 * g + 512)
                sq0 = sqb[2 * (g % 2)]
                sq1 = sqb[2 * (g % 2) + 1]
                n0 = "gsq%d" % (2 * (g % 2))
                n1 = "gsq%d" % (2 * (g % 2) + 1)
                ACT(sq0, YT[:, 0, sl], AF.Square, r=["YT0_%d" % g], w=[n0])
                ACT(sq1, YT[:, 1, sl], AF.Square, r=["YT1_%d" % g], w=[n1])
                pb = psn()
                MM(pb.ap, onesb[:], sq0, True, False, r=[n0], w=[pb.name])
                MM(pb.ap, onesb[:], sq1, False, True, r=[n1], w=[pb.name])
                gheld[g] = pb

            def GQ(g):
                sl = slice(512 * g, 512 * g + 512)
                pb = gheld.pop(g)
                rr = lrs[g % 2]
                rn = "grr%d" % (g % 2)
                ACT(rr, pb.ap, AF.Ln, r=[pb.name], w=[rn], scale=1.0 / 256, bias=EPS)
                ACT(rr, rr, AF.Exp, r=[rn], w=[rn], scale=-0.5)
                for c in range(2):
                    STT(YT[:, c, sl], YT[:, c, sl], pc(l, 16 + 2 * gi + c), rr, ALU.mult, ALU.mult,
                        r=["YT%d_%d" % (c, g), rn], w=["YT%d_%d" % (c, g)])

            GP(0)
            for g in range(4):
                if g + 1 < 4:
                    GP(g + 1)
                GQ(g)
            for g in range(4):
                for i in range(4):
                    t = 4 * g + i
                    for h in range(2):
                        py = psn()
                        for c in range(2):
                            MM(py.ap, YT[:, c, 128 * t:128 * t + 128], w3[:, c, 512 * h:512 * h + 512], c == 0, c == 1,
                               r=["YT%d_%d" % (c, g)] + slot.names, w=[py.name])
                        xs_ = X[:, t, 512 * h:512 * h + 512]
                        TT(xs_, py.ap, xs_, ALU.add, r=[py.name, "X%d" % t], w=["X%d" % t])

        def phase_attn(l, pr):
            slot = ws_next()
            wv3 = slot.ap[:, 0:3072].rearrange("p (c n) -> p c n", c=8)
            for j, c0 in enumerate([128 * pr, 256 + 128 * pr, 512 + 128 * pr]):
                P.dma(wv3[:, :, 128 * j:128 * j + 128], w_in_view(l)[:, :, c0:c0 + 128], slot.name, w=slot.names, eng="pool")
            yield
            A.reset()
            QTh = [A.bf16(2048) for _ in range(2)]
            KT = A.bf16(2048)
            Vpf = A.bf16(4096)
            Vp = Vpf.rearrange("p (s h n) -> p s h n", s=16, h=2)
            eb = [A.f32(512) for _ in range(5)]
            spb = [SPR[:, 512 * i:512 * i + 512].bitcast(F32) for i in range(5)]
            Rb = [SPR[:, 512 * i:512 * i + 512].bitcast(F32) for i in range(5, 10)]
            wb = [A.bf16(512) for _ in range(4)]
            sqs = [A.bf16(512) for _ in range(2)]
            rrs = [A.f32(512) for _ in range(2)]
            assert A.off <= 10240
            MS(Vpf, 0.0, w=["Vp%d" % g for g in range(4)])
            MS(QTh[0][64:128, :], 0.0, w=["QTz"])
            MS(QTh[1][0:64, :], 0.0, w=["QTz"])
            items = [(g, wi) for g in range(4) for wi in range(2)]
            held = {}

            def PJ(i):
                g, wi = items[i]
                sl = slice(512 * g, 512 * g + 512)
                pa = psn()
                for dc in range(8):
                    MM(pa.ap, wv3[:, dc, 128 * wi:128 * wi + 128], HT[:, dc, sl], dc == 0, dc == 7,
                       r=slot.names + ["HT%d" % g], w=[pa.name])
                sq_ = sqs[i % 2]
                sqn = "asq%d" % (i % 2)
                ACT(sq_, pa.ap, AF.Square, r=[pa.name], w=[sqn])
                pk = psn()
                MM(pk.ap, blkonesb[:], sq_, True, True, r=[sqn], w=[pk.name])
                held[i] = (pa, pk)

            def QJ(i):
                g, wi = items[i]
                sl = slice(512 * g, 512 * g + 512)
                pa, pk = held.pop(i)
                rr = rrs[i % 2]
                rn = "arr%d" % (i % 2)
                ACT(rr, pk.ap, AF.Ln, r=[pk.name], w=[rn], scale=1.0 / 64, bias=EPS)
                ACT(rr, rr, AF.Exp, r=[rn], w=[rn], scale=-0.5)
                if wi == 0:
                    for h in range(2):
                        hp = slice(64 * h, 64 * h + 64)
                        STT(QTh[h][hp, sl], pa.ap[hp, :], qk8[hp, l:l + 1], rr[hp, :], ALU.mult, ALU.mult,
                            r=[pa.name, rn, "QTz"], w=["QT%d" % g])
                else:
                    STT(KT[:, sl], pa.ap, pc(l, 39), rr, ALU.mult, ALU.mult, r=[pa.name, rn], w=["KT%d" % g])

            def VJ(g):
                pv = psn()
                for i in range(4):
                    t = 4 * g + i
                    for dc in range(8):
                        MM(pv.ap[:, 128 * i:128 * i + 128], HT[:, dc, 128 * t:128 * t + 128], wv3[:, dc, 256:384],
                           dc == 0, dc == 7, r=slot.names + ["HT%d" % g], w=[pv.name])
                pv3 = pv.ap.rearrange("p (i n) -> p i n", i=4)
                ACT(Vp[:, 4 * g:4 * g + 4, 0, 0:64], pv3[:, :, 0:64], AF.Copy, r=[pv.name], w=["Vp%d" % g])
                CP(Vp[:, 4 * g:4 * g + 4, 1, 64:128], pv3[:, :, 64:128], r=[pv.name], w=["Vp%d" % g])

            PJ(0)
            for i in range(len(items)):
                if i + 1 < len(items):
                    PJ(i + 1)
                QJ(i)
                if i % 2 == 1:
                    VJ(i // 2)
            blocks = []
            for qt in range(4):
                for h in range(2):
                    jmax = 4 * qt + 3
                    for j in range(jmax, -1, -1):
                        blocks.append(dict(qt=qt, h=h, j=j, k=j - 4 * qt, pos=jmax - j,
                                           first=(h == 0 and j == jmax), last=(h == 1 and j == 0)))
            for n, b in enumerate(blocks):
                b["n"] = n
            pos = {}

            def zmm(pt, b, stop_):
                qs = slice(512 * b["qt"], 512 * b["qt"] + 512)
                ks = slice(128 * b["j"], 128 * b["j"] + 128)
                rq = ["KT%d" % (b["j"] // 4), "QT%d" % b["qt"]]
                k = b["k"]
                MM(pt.ap, KT[:, ks], QTh[b["h"]][:, qs], True, stop_ and k < 0, r=rq, w=[pt.name])
                if k >= 0:
                    MM(pt.ap, identb[:], NEGW[:, 384 - 128 * k:384 - 128 * k + 512], False, stop_, r=[], w=[pt.name])

            def S1(b):
                n = b["n"]
                pz = psn()
                zmm(pz, b, True)
                e_ = eb[n % 5]
                en = "ae%d" % (n % 5)
                ACT(e_, pz.ap, AF.Exp, r=[pz.name], w=[en])
                sp = spb[n % 5]
                spn = "sp%d" % (n % 5)
                ACT(sp.bitcast(F32R), e_, AF.Ln, r=[en], w=[spn], bias=1.0)
                if b["pos"] == 0:
                    b["R"] = None
                else:
                    prev = blocks[n - 1]
                    psp = spb[(n - 1) % 5]
                    pspn = "sp%d" % ((n - 1) % 5)
                    nr = Rb[n % 5]
                    nrn = "aR%d" % (n % 5)
                    if prev["R"] is None:
                        P.op("dve", lambda e: e.tensor_copy(out=nr.bitcast(F32R), in_=psp), r=[pspn], w=[nrn])
                    else:
                        TT(nr.bitcast(F32R), prev["R"].ap, psp, ALU.add, r=[prev["R"].name, pspn], w=[nrn])
                    b["R"] = Res(nr, nrn)

            def S2(b):
                n = b["n"]
                sp = spb[n % 5]
                spn = "sp%d" % (n % 5)
                R = b["R"]
                pb = psn()
                zmm(pb, b, False)
                MM(pb.ap, ntri[:], sp.bitcast(F32R), False, R is None, r=[spn], w=[pb.name])
                if R is not None:
                    MM(pb.ap, nones[:], R.ap.bitcast(F32R), False, True, r=[R.name], w=[pb.name])
                w_ = wb[n % 4]
                wn = "aw%d" % (n % 4)
                ACT(w_, pb.ap, AF.Exp, r=[pb.name], w=[wn])

            def S3(b):
                n = b["n"]
                qs = slice(512 * b["qt"], 512 * b["qt"] + 512)
                if b["first"]:
                    pos["po"] = psl()
                po = pos["po"]
                w_ = wb[n % 4]
                wn = "aw%d" % (n % 4)
                MM(po.ap, Vp[:, b["j"], b["h"], :], w_, b["first"], b["last"], r=["Vp%d" % (b["j"] // 4), wn], w=[po.name])
                if b["last"]:
                    ACT(YT[:, pr, qs], po.ap, AF.Copy, r=[po.name], w=["YT%d_%d" % (pr, b["qt"])])

            LOOK = 2
            for i in range(len(blocks) + 2 * LOOK):
                if i < len(blocks):
                    S1(blocks[i])
                if 0 <= i - LOOK < len(blocks):
                    S2(blocks[i - LOOK])
                if 0 <= i - 2 * LOOK < len(blocks):
                    S3(blocks[i - 2 * LOOK])
            if pr == 1:
                P.fence()
                group_norm_out(l, 0)

        def phase_conf(l):
            slot = ws_next()
            wv3 = slot.ap[:, 0:4096].rearrange("p (c n) -> p c n", c=8)
            P.dma(wv3, w_in_view(l)[:, :, 768:1280], slot.name, w=slot.names, eng="pool")
            yield
            A.reset()
            hpad = A.bf16(4160).rearrange("p (c n) -> p c n", c=2)
            dg = A.bf16(7936).rearrange("p (c k n) -> p c k n", c=2, k=31)
            sig = A.f32(512)
            cv = A.f32(1024).rearrange("p (c n) -> p c n", c=2)
            sqv = A.f32(1024).rearrange("p (c n) -> p c n", c=2)
            mean = A.f32(512)
            tmp = A.f32(512)
            rr = A.f32(512)
            for c in range(2):
                MS(hpad[:, c, 0:30], 0.0, w=["hp%d" % c])
                for k in range(31):
                    TS(dg[:, c, k, :], identf[:], pc(l, 40 + 31 * c + k), None, ALU.mult, None, r=[], w=["dg"])
            for g in range(4):
                sl = slice(512 * g, 512 * g + 512)
                for c in range(2):
                    pv = psn()
                    pg = psn()
                    for dc in range(8):
                        MM(pv.ap, wv3[:, dc, 128 * c:128 * c + 128], HT[:, dc, sl], dc == 0, dc == 7, r=slot.names + ["HT%d" % g], w=[pv.name])
                    for dc in range(8):
                        MM(pg.ap, wv3[:, dc, 256 + 128 * c:256 + 128 * c + 128], HT[:, dc, sl], dc == 0, dc == 7, r=slot.names + ["HT%d" % g], w=[pg.name])
                    ACT(sig, pg.ap, AF.Sigmoid, r=[pg.name], w=["csig"])
                    TT(hpad[:, c, 30 + 512 * g:30 + 512 * g + 512], pv.ap, sig, ALU.mult, r=[pv.name, "csig"], w=["hp%d" % c])
            for g in range(4):
                sl = slice(512 * g, 512 * g + 512)
                for c in range(2):
                    pcv = psn()
                    for k in range(31):
                        MM(pcv.ap, dg[:, c, k, :], hpad[:, c, 512 * g + k:512 * g + k + 512], k == 0, k == 30, r=["dg", "hp%d" % c], w=[pcv.name])
                    ACT(cv[:, c, :], pcv.ap, AF.Identity, r=[pcv.name], w=["cv%d" % c], bias=pc(l, 24 + c))
                    ACT(sqv[:, c, :], cv[:, c, :], AF.Square, r=["cv%d" % c], w=["csq%d" % c])
                p1 = psn()
                p2 = psn()
                for c in range(2):
                    MM(p1.ap, onesf[:], cv[:, c, :], c == 0, c == 1, r=["cv%d" % c], w=[p1.name])
                for c in range(2):
                    MM(p2.ap, onesf[:], sqv[:, c, :], c == 0, c == 1, r=["csq%d" % c], w=[p2.name])
                TS(mean, p1.ap, 1.0 / 256, None, ALU.mult, None, r=[p1.name], w=["cmean"])
                TT(tmp, mean, mean, ALU.mult, r=["cmean"], w=["ctmp"])
                STT(tmp, p2.ap, 1.0 / 256, tmp, ALU.mult, ALU.subtract, r=[p2.name, "ctmp"], w=["ctmp"])
                ACT(tmp, tmp, AF.Ln, r=["ctmp"], w=["ctmp"], bias=EPS)
                ACT(rr, tmp, AF.Exp, r=["ctmp"], w=["crr"], scale=-0.5)
                for c in range(2):
                    TT(cv[:, c, :], cv[:, c, :], mean, ALU.subtract, r=["cv%d" % c, "cmean"], w=["cv%d" % c])
                    TT(cv[:, c, :], cv[:, c, :], rr, ALU.mult, r=["cv%d" % c, "crr"], w=["cv%d" % c])
                    ACT(YT[:, c, sl], cv[:, c, :], AF.Silu, r=["cv%d" % c], w=["YT%d_%d" % (c, g)],
                        scale=pc(l, 26 + c), bias=pc(l, 28 + c))
            group_norm_out(l, 1)

        def phase_gmlp(l):
            slot = ws_next()
            wv3 = slot.ap[:, 0:4096].rearrange("p (c n) -> p c n", c=8)
            P.dma(wv3, w_in_view(l)[:, :, 1280:1792], slot.name, w=slot.names, eng="pool")
            yield
            A.reset()
            uT = A.bf16(4096).rearrange("p (c n) -> p c n", c=2)
            vpf = A.bf16(8192)
            vp = vpf.rearrange("p (s h n) -> p s h n", s=16, h=4)
            WTr = A.bf16(512).rearrange("p (h t) -> p h t", h=4)
            WTm = A.bf16(512).rearrange("p (h t) -> p h t", h=4)
            bsT = A.f32(256).rearrange("p (c t) -> p c t", c=2)
            bs4 = A.f32(1024).rearrange("p (c q t) -> p c q t", c=2, q=4)
            gb = A.f32(512)
            gvb = [A.f32(256) for _ in range(2)]
            vnb = [A.f32(256) for _ in range(2)]
            tmp = A.f32(512)
            P.dma(WTr, wsT_d[l].rearrange("p (h t) -> p h t", h=4), "gWT", w=["gWTr"], eng="pool")
            P.dma(bsT, bsT_d[l].rearrange("p (c t) -> p c t", c=2), "gbs", w=["gbsT"])
            P.dma(gb, pbc_d[l][:, 0:512].partition_broadcast(128), "ggb", w=["ggb"])
            MS(vpf, 0.0, w=["gvp"])
            for h in range(4):
                TT(WTm[:, h, :], WTr[:, h, :], maskle[:], ALU.mult, r=["gWTr"], w=["gWTm"])
            for c in range(2):
                for q in range(4):
                    CP(bs4[:, c, q, :], bsT[:, c, :], r=["gbsT"], w=["gbs4"])
            for g in range(4):
                sl = slice(512 * g, 512 * g + 512)
                for c in range(2):
                    pu = psn()
                    for dc in range(8):
                        MM(pu.ap, wv3[:, dc, 128 * c:128 * c + 128], HT[:, dc, sl], dc == 0, dc == 7, r=slot.names + ["HT%d" % g], w=[pu.name])
                    ACT(uT[:, c, sl], pu.ap, AF.Gelu, r=[pu.name], w=["guT%d" % g])
            mv = small[:, 420:452].rearrange("p (t k) -> p t k", k=2)
            lnb = small[:, 452:468]
            rsb = small[:, 468:484]
            for t in range(NT):
                g = t // 4
                pv = psn()
                for dc in range(8):
                    MM(pv.ap[:, 0:256], HT[:, dc, 128 * t:128 * t + 128], wv3[:, dc, 256:512], dc == 0, dc == 7, r=slot.names + ["HT%d" % g], w=[pv.name])
                gv = gvb[t % 2]
                gvn = "ggv%d" % (t % 2)
                ACT(gv, pv.ap[:, 0:256], AF.Gelu, r=[pv.name], w=[gvn])
                P.op("dve", lambda e, gv=gv: e.bn_stats(out=bnst[:, 0:6], in_=gv), r=[gvn], w=["gbn6"])
                P.op("dve", lambda e, t=t: e.bn_aggr(out=mv[:, t, :], in_=bnst[:, 0:6]), r=["gbn6"], w=["gmv"])
                gv3 = gv.rearrange("p (h d) -> p h d", d=64)
                for hh in range(2):
                    CP(vp[:, t, hh::2, 64 * hh:64 * hh + 64], gv3[:, hh::2, :], r=[gvn, "gvp"], w=["gvp%d" % t])
            ACT(lnb, mv[:, :, 1], AF.Ln, r=["gmv"], w=["glnb"], bias=EPS)
            ACT(rsb, lnb, AF.Exp, r=["glnb"], w=["grsb"], scale=-0.5)
            g3 = gb[:, 0:256].rearrange("p (h d) -> p h d", d=64)
            b3 = gb[:, 256:512].rearrange("p (h d) -> p h d", d=64)
            for t in range(NT):
                for hh in range(2):
                    v_ = vp[:, t, hh::2, 64 * hh:64 * hh + 64]
                    TS(v_, v_, mv[:, t, 0:1], rsb[:, t:t + 1], ALU.subtract, ALU.mult, r=["gvp%d" % t, "gmv", "grsb"], w=["gvp%d" % t])
                    TT(v_, v_, g3[:, hh::2, :], ALU.mult, r=["gvp%d" % t, "ggb"], w=["gvp%d" % t])
                    TT(v_, v_, b3[:, hh::2, :], ALU.add, r=["gvp%d" % t, "ggb"], w=["gvp%d" % t])
            for g in range(4):
                sl = slice(512 * g, 512 * g + 512)
                for c in range(2):
                    pm = psn()
                    for q in range(4):
                        cb = 4 * g + q
                        for hh in range(2):
                            h = 2 * c + hh
                            MM(pm.ap[:, 128 * q:128 * q + 128], vp[:, cb, h, :], WTm[:, h, :], hh == 0, hh == 1, r=["gvp%d" % cb, "gWTm"], w=[pm.name])
                    TT(tmp, pm.ap, bs4[:, c, :, :].rearrange("p q t -> p (q t)"), ALU.add, r=[pm.name, "gbs4"], w=["gtmp"])
                    TT(YT[:, c, sl], tmp, uT[:, c, sl], ALU.mult, r=["gtmp", "guT%d" % g], w=["YT%d_%d" % (c, g)])
            group_norm_out(l, 2)

        def phase_lru(l):
            slot = ws_next()
            wv3 = slot.ap[:, 0:4096].rearrange("p (c n) -> p c n", c=8)
            P.dma(wv3, w_in_view(l)[:, :, 1792:2304], slot.name, w=slot.names, eng="pool")
            yield
            A.reset()
            xpad = A.f32(2052)
            xb = A.f32(2048)
            xbb = A.bf16(2048)
            gl = A.bf16(2048)
            bdf = A.bf16(512)
            bd = bdf.rearrange("p (a c n) -> p a c n", a=2, c=2)
            hq = [A.f32(512) for _ in range(2)]
            r_ = A.f32(512)
            i_ = A.f32(512)
            a_ = A.f32(512)
            s_ = A.f32(512)
            b_ = A.f32(512)
            MS(bdf, 0.0, w=["lbd"])
            for a, src in enumerate([wa_d, wx_d]):
                for c in range(2):
                    for hh in range(2):
                        P.dma(bd[64 * hh:64 * hh + 64, a, c, 64 * hh:64 * hh + 64], src[l, 2 * c + hh], "lbd", r=[], w=["lbd"], eng="pool")
            ACT(clc[:, 4:6], pc(l, 36, 2), AF.Exp, r=[], w=["lcl_t"], scale=-1.0)
            ACT(clc[:, 6:8], clc[:, 4:6], AF.Ln, r=["lcl_t"], w=["lcl_u"], bias=1.0)
            TS(clc[:, 0:2], clc[:, 6:8], -8.0, None, ALU.mult, None, r=["lcl_u"], w=["lcl"])
            TS(clc[:, 2:4], clc[:, 6:8], -16.0, None, ALU.mult, None, r=["lcl_u"], w=["lcl"])
            for c in range(2):
                MS(xpad[:, 0:3], 0.0, w=["lxp"])
                for g in range(4):
                    sl = slice(512 * g, 512 * g + 512)
                    px = psn()
                    pg = psn()
                    for dc in range(8):
                        MM(px.ap, wv3[:, dc, 128 * c:128 * c + 128], HT[:, dc, sl], dc == 0, dc == 7, r=slot.names + ["HT%d" % g], w=[px.name])
                    for dc in range(8):
                        MM(pg.ap, wv3[:, dc, 256 + 128 * c:256 + 128 * c + 128], HT[:, dc, sl], dc == 0, dc == 7, r=slot.names + ["HT%d" % g], w=[pg.name])
                    ACT(xpad[:, 3 + 512 * g:3 + 512 * g + 512], px.ap, AF.Copy, r=[px.name], w=["lxp"])
                    ACT(gl[:, sl], pg.ap, AF.Gelu, r=[pg.name], w=["lgl"])
                TS(xb, xpad[:, 0:2048], pc(l, 102 + 4 * c), pc(l, 30 + c), ALU.mult, ALU.add, r=["lxp"], w=["lxb"])
                for k in range(1, 4):
                    STT(xb, xpad[:, k:k + 2048], pc(l, 102 + 4 * c + k), xb, ALU.mult, ALU.add, r=["lxp", "lxb"], w=["lxb"])
                ACT(xbb, xb, AF.Copy, r=["lxb"], w=["lxbb"])
                for g in range(4):
                    sl = slice(512 * g, 512 * g + 512)
                    pr_ = psn()
                    pi_ = psn()
                    MM(pr_.ap, bd[:, 0, c, :], xbb[:, sl], True, True, r=["lbd", "lxbb"], w=[pr_.name])
                    MM(pi_.ap, bd[:, 1, c, :], xbb[:, sl], True, True, r=["lbd", "lxbb"], w=[pi_.name])
                    ACT(r_, pr_.ap, AF.Sigmoid, r=[pr_.name], w=["lr"], bias=pc(l, 32 + c))
                    ACT(i_, pi_.ap, AF.Sigmoid, r=[pi_.name], w=["li"], bias=pc(l, 34 + c))
                    ACT(a_, r_, AF.Exp, r=["lr", "lcl"], w=["la"], scale=clc[:, c:c + 1])
                    ACT(s_, r_, AF.Exp, r=["lr", "lcl"], w=["ls"], scale=clc[:, 2 + c:3 + c])
                    ACT(s_, s_, AF.Sqrt, r=["ls"], w=["ls"], scale=-1.0, bias=1.0)
                    TT(b_, i_, xb[:, sl], ALU.mult, r=["li", "lxb"], w=["lb"])
                    TT(b_, b_, s_, ALU.mult, r=["lb", "ls"], w=["lb"])
                    hcur = hq[g % 2]
                    hprev = hq[(g + 1) % 2]
                    init = 0.0 if g == 0 else hprev[:, 511:512]
                    rd = ["la", "lb"] + ([] if g == 0 else ["lh%d" % ((g + 1) % 2)])
                    P.op("dve", lambda e, hcur=hcur, init=init: e.tensor_tensor_scan(out=hcur, data0=a_, data1=b_, initial=init, op0=ALU.mult, op1=ALU.add),
                         r=rd, w=["lh%d" % (g % 2)])
                    TT(YT[:, c, sl], hcur, gl[:, sl], ALU.mult, r=["lh%d" % (g % 2), "lgl"], w=["YT%d_%d" % (c, g)])
            group_norm_out(l, 3)

        def phase_router(l):
            yield
            A.reset()
            rfm = A.f32(64).rearrange("p (c e) -> p c e", c=8)
            Rg = A.f32(64).rearrange("p (c e) -> p c e", c=8)
            hn = [A.f32(1024) for _ in range(2)]
            hT = [A.f32(1024) for _ in range(2)]
            P.dma(rfm, routerF_d.rearrange("p (c e) -> p c e", c=8), "rtB", w=["rfm"])
            for dc in range(8):
                TS(Rg[:, dc, :], rfm[:, dc, :], pc(l, 8 + dc), None, ALU.mult, None, r=["rfm"], w=["Rg"])
            for t in range(NT):
                h_ = hn[t % 2]
                hnn = "rhn%d" % (t % 2)
                hT_ = hT[t % 2]
                TS(h_, X[:, t, :], rs[:, t:t + 1], None, ALU.mult, None, r=["X%d" % t, "rs"], w=[hnn])
                for half in range(2):
                    pb = psn()
                    for i in range(4):
                        dc = 4 * half + i
                        P.op("pe", lambda e, o=pb.ap[:, 128 * i:128 * i + 128], a=h_[:, 128 * dc:128 * dc + 128]:
                             e.transpose(out=o, in_=a, identity=identf[:]), r=[hnn], w=[pb.name])
                    htn = "rhT%d_%d" % (t % 2, half)
                    if half == 0:
                        ACT(hT_[:, 0:512], pb.ap, AF.Copy, r=[pb.name], w=[htn])
                    else:
                        CP(hT_[:, 512:1024], pb.ap, r=[pb.name], w=[htn])
                pl = psn()
                for dc in range(8):
                    MM(pl.ap[:, 0:8], hT_[:, 128 * dc:128 * dc + 128], Rg[:, dc, :], dc == 0, dc == 7,
                       r=["rhT%d_%d" % (t % 2, dc // 4), "Rg"], w=[pl.name])
                CP(logits[:, t, :], pl.ap[:, 0:8], r=[pl.name], w=["rlg%d" % t])
                lg = logits[:, t, :]
                m1 = tk[:, 0:1]
                m2 = tk[:, 1:2]
                dd = tk[:, 2:3]
                g1 = tk[:, 3:4]
                g2 = tk[:, 4:5]
                eq1 = tk[:, 8:16]
                eq2 = tk[:, 16:24]
                l2 = tk[:, 24:32]
                P.op("dve", lambda e, lg=lg: e.reduce_max(out=m1, in_=lg, axis=AX.X), r=["rlg%d" % t], w=["rm1"])
                TS(eq1, lg, m1, None, ALU.is_equal, None, r=["rlg%d" % t, "rm1"], w=["req1"])
                STT(l2, eq1, -1e30, lg, ALU.mult, ALU.add, r=["req1", "rlg%d" % t], w=["rl2"])
                P.op("dve", lambda e: e.reduce_max(out=m2, in_=l2, axis=AX.X), r=["rl2"], w=["rm2"])
                TS(eq2, l2, m2, None, ALU.is_equal, None, r=["rl2", "rm2"], w=["req2"])
                TT(dd, m1, m2, ALU.subtract, r=["rm1", "rm2"], w=["rdd"])
                ACT(g1, dd, AF.Sigmoid, r=["rdd"], w=["rg1"])
                ACT(g2, dd, AF.Sigmoid, r=["rdd"], w=["rgg2"], scale=-1.0)
                TS(gates[:, t, :], eq1, g1, None, ALU.mult, None, r=["req1", "rg1"], w=["gates"])
                STT(gates[:, t, :], eq2, g2, gates[:, t, :], ALU.mult, ALU.add, r=["req2", "rgg2", "gates"], w=["gates"])

        def phase_ffn(l, moe):
            if moe:
                groups = [(e, f) for e in range(NE) for f in range(DFE // 256)]
            else:
                groups = [(0, f) for f in range(DFF // 256)]

            def load_up(n, slot):
                e, f = groups[n]
                gsrc = mg_d[0, e] if moe else fg_d[0]
                usrc = mu_d[0, e] if moe else fu_d[0]
                for j, src in enumerate([gsrc, usrc]):
                    P.dma(slot.ap[:, 2048 * j:2048 * j + 2048].rearrange("p (c n) -> p c n", c=8),
                          src.rearrange("(c p) n -> p c n", p=128)[:, :, 256 * f:256 * f + 256],
                          slot.names[0], w=[slot.names[0]], eng="pool")

            def load_dn(n, slot):
                e, f = groups[n]
                dsrc = md_d[0, e] if moe else fd_d[0]
                P.dma(slot.ap[:, 4096:6144].rearrange("p (c n) -> p c n", c=2),
                      dsrc[256 * f:256 * f + 256, :].rearrange("(c p) n -> p c n", p=128),
                      slot.names[1], w=[slot.names[1]], eng="pool")

            base = cnt["ws"]
            cnt["ws"] += len(groups)

            def slot_of(n):
                return ws_slot((base + n) % 2)

            s0 = slot_of(0)
            load_up(0, s0)
            load_dn(0, s0)
            yield
            A.reset()
            AT = [A.bf16(4096).rearrange("p (c n) -> p c n", c=2) for _ in range(2)]
            sgb = [A.f32(512) for _ in range(2)]
            if len(groups) > 1:
                s1 = slot_of(1)
                load_up(1, s1)
                load_dn(1, s1)
            k = [0]

            def up(n):
                slot = slot_of(n)
                at = AT[n % 2]
                wg3 = slot.ap[:, 0:2048].rearrange("p (c n) -> p c n", c=8)
                wu3 = slot.ap[:, 2048:4096].rearrange("p (c n) -> p c n", c=8)
                for g in range(4):
                    sl = slice(512 * g, 512 * g + 512)
                    for fc in range(2):
                        pg = psn()
                        pu = psn()
                        for dc in range(8):
                            MM(pg.ap, wg3[:, dc, 128 * fc:128 * fc + 128], HT[:, dc, sl], dc == 0, dc == 7, r=[slot.names[0], "HT%d" % g], w=[pg.name])
                        for dc in range(8):
                            MM(pu.ap, wu3[:, dc, 128 * fc:128 * fc + 128], HT[:, dc, sl], dc == 0, dc == 7, r=[slot.names[0], "HT%d" % g], w=[pu.name])
                        sg = sgb[k[0] % 2]
                        sgn = "fsg%d" % (k[0] % 2)
                        k[0] += 1
                        ACT(sg, pg.ap, AF.Silu, r=[pg.name], w=[sgn])
                        TT(at[:, fc, sl], sg, pu.ap, ALU.mult, r=[sgn, pu.name], w=["fAT%d" % (n % 2)])
                        yield

            def down(n):
                e, f = groups[n]
                slot = slot_of(n)
                at = AT[n % 2]
                wd3 = slot.ap[:, 4096:6144].rearrange("p (c n) -> p c n", c=2)
                for t in range(NT):
                    for h in range(2):
                        py = psn()
                        for fc in range(2):
                            MM(py.ap, at[:, fc, 128 * t:128 * t + 128], wd3[:, fc, 512 * h:512 * h + 512], fc == 0, fc == 1,
                               r=["fAT%d" % (n % 2), slot.names[1]], w=[py.name])
                        xs_ = X[:, t, 512 * h:512 * h + 512]
                        if moe:
                            STT(xs_, py.ap, gates[:, t, e:e + 1], xs_, ALU.mult, ALU.add, r=[py.name, "X%d" % t, "gates"], w=["X%d" % t])
                        else:
                            TT(xs_, py.ap, xs_, ALU.add, r=[py.name, "X%d" % t], w=["X%d" % t])
                        yield

            N = len(groups)
            for _ in up(0):
                pass
            for n in range(N):
                ug = up(n + 1) if n + 1 < N else None
                dg = down(n)
                for step in range(8):
                    if ug is not None:
                        next(ug, None)
                    for _ in range(4):
                        next(dg, None)
                if ug is not None:
                    for _ in ug:
                        pass
                for _ in dg:
                    pass
                if n + 2 < N:
                    load_up(n + 2, slot_of(n + 2))
                    load_dn(n + 2, slot_of(n + 2))

        gens = []
        for sq in range(nseq):
            gens.append(phase_load(sq))
            for l in layers:
                gens.append(phase_norm(l, 0))
                gens.append(phase_attn(l, 0))
                gens.append(phase_attn(l, 1))
                gens.append(phase_conf(l))
                gens.append(phase_gmlp(l))
                gens.append(phase_lru(l))
                gens.append(phase_norm(l, 8))
                if l % 2 == 1:
                    gens.append(phase_router(l))
                gens.append(phase_ffn(l, l % 2 == 1))
            gens.append(phase_store(sq))
        if n_phases is not None:
            gens = gens[:n_phases] + [phase_store(0)]
        next(gens[0])
        for i, g in enumerate(gens):
            for _ in g:
                pass
            if i + 1 < len(gens):
                next(gens[i + 1])
            P.fence()
        P.wait_all("sp", stores)
        P.emit(st)
    return nc


def prep_shared(inp):
    f = lambda a: np.ascontiguousarray(np.asarray(a, dtype=np.float32))
    pcol = np.zeros((128, L * NPC), np.float32)

    def col(v, n):
        return np.asarray(v, np.float32).reshape(n, 128).T

    for l in range(L):
        b = l * NPC
        pcol[:, b + 0:b + 8] = col(inp["norm1_g"][l], 8)
        pcol[:, b + 8:b + 16] = col(inp["norm2_g"][l], 8)
        pcol[:, b + 16:b + 24] = col(inp["group_norm_g"][l], 8)
        pcol[:, b + 24:b + 26] = col(inp["conf_dw_b"][l], 2)
        pcol[:, b + 26:b + 28] = col(inp["conf_ln_g"][l], 2)
        pcol[:, b + 28:b + 30] = col(inp["conf_ln_b"][l], 2)
        pcol[:, b + 30:b + 32] = col(inp["lru_conv_b"][l], 2)
        pcol[:, b + 32:b + 34] = col(inp["lru_ba"][l], 2)
        pcol[:, b + 34:b + 36] = col(inp["lru_bx"][l], 2)
        pcol[:, b + 36:b + 38] = col(inp["lru_lambda"][l], 2)
        pcol[:, b + 38] = np.tile(np.asarray(inp["q_norm_g"][l], np.float32), 2)
        pcol[:, b + 39] = np.tile(np.asarray(inp["k_norm_g"][l], np.float32), 2)
        cw = np.asarray(inp["conf_dw_w"][l], np.float32)
        lw = np.asarray(inp["lru_conv_w"][l], np.float32)
        for c in range(2):
            pcol[:, b + 40 + 31 * c:b + 40 + 31 * c + 31] = cw[:, 128 * c:128 * c + 128].T
            pcol[:, b + 102 + 4 * c:b + 102 + 4 * c + 4] = lw[:, 128 * c:128 * c + 128].T
    pbc = np.zeros((L, 1, 1536), np.float32)
    wsT = np.zeros((L, 128, 512), np.float32)
    bsT = np.zeros((L, 128, 256), np.float32)
    for l in range(L):
        pbc[l, 0, 0:256] = inp["gmlp_ln_g"][l]
        pbc[l, 0, 256:512] = inp["gmlp_ln_b"][l]
        pbc[l, 0, 512:1536] = inp["norm2_g"][l]
        wsT[l] = np.asarray(inp["gmlp_ws"][l], np.float32).transpose(2, 0, 1).reshape(128, 512)
        bsT[l] = np.repeat(np.asarray(inp["gmlp_bs"][l], np.float32), 64, axis=0).reshape(2, 128, 128).transpose(1, 0, 2).reshape(128, 256)
    routerF = np.ascontiguousarray(np.asarray(inp["moe_router"][0], np.float32).reshape(8, 128, NE).transpose(1, 0, 2)).reshape(128, 8 * NE)
    return {
        "w_in": f(inp["w_in"]), "w_out": f(inp["w_out"]),
        "ffn_w_gate": f(inp["ffn_w_gate"]), "ffn_w_up": f(inp["ffn_w_up"]), "ffn_w_down": f(inp["ffn_w_down"]),
        "moe_w_gate": f(inp["moe_w_gate"]), "moe_w_up": f(inp["moe_w_up"]), "moe_w_down": f(inp["moe_w_down"]),
        "pcol": pcol, "pbc": pbc, "wsT": wsT, "bsT": bsT, "routerF": routerF,
        "lru_wa": f(inp["lru_wa"]), "lru_wx": f(inp["lru_wx"]),
    }


def kernel(**inputs):
    x = np.ascontiguousarray(np.asarray(inputs["x"], dtype=np.float32))
    B = x.shape[0]
    nseq = B // N_CORES
    shared = prep_shared(inputs)
    nc = build_nc(nseq=nseq)
    in_maps = []
    for c in range(N_CORES):
        m = dict(shared)
        m["x"] = x[c * nseq:(c + 1) * nseq]
        in_maps.append(m)
    res = run_bass_kernel_spmd(nc, in_maps, core_ids=list(range(N_CORES)))
    out = np.concatenate([np.asarray(r["y"], dtype=np.float32) for r in res.results], axis=0)
    return out
```

```python
import numpy as np
from contextlib import ExitStack
import concourse.bass as bass
import concourse.mybir as mybir
from concourse.bass_utils import run_bass_kernel_spmd

F32 = mybir.dt.float32
BF16 = mybir.dt.bfloat16
I32 = mybir.dt.int32
F32R = mybir.dt.float32r
AF = mybir.ActivationFunctionType
ALU = mybir.AluOpType
AX = mybir.AxisListType

D = 1024
S = 2048
NT = 16
L = 2
INC = 2304
DFF = 2816
NE = 8
DFE = 3584
EPS = 1e-6
NPC = 110
N_CORES = 8


class Prog:
    def __init__(self, nc):
        self.nc = nc
        self.ops = []
        self.last_w = {}
        self.readers = {}
        self.dma_cnt = {}
        self.dma_last = {}
        self.dma_arena = {}
        self.fence_deps = {}
        self.fence_pending = set()

    def _deps(self, r, w):
        deps = {}
        for x in r:
            lw = self.last_w.get(x)
            if lw is not None:
                deps[lw] = True
        for x in w:
            lw = self.last_w.get(x)
            if lw is not None:
                deps.setdefault(lw, False)
            for rd in self.readers.get(x, ()):
                deps.setdefault(rd, False)
        i = len(self.ops)
        for x in r:
            self.readers.setdefault(x, []).append(i)
        for x in w:
            self.last_w[x] = i
            self.readers[x] = []
        return deps

    @staticmethod
    def _persistent(name):
        return name.startswith(("X", "HT", "YT", "WS", "ps"))

    def _fence_check(self, eng, r, w, deps):
        arena = any(not self._persistent(x) for x in r) or any(not self._persistent(x) for x in w)
        if arena and eng in self.fence_pending:
            self.fence_pending.discard(eng)
            for i in self.fence_deps:
                deps[i] = True
        return arena

    def fence(self):
        last = {}
        for i, o in enumerate(self.ops):
            if o["dma"] is None and o["fn"] is not None:
                last[o["eng"]] = i
        deps = {i: True for i in last.values()}
        for k, i in self.dma_last.items():
            if self.dma_arena.get(k):
                deps[i] = True
        self.fence_deps = deps
        self.fence_pending = set(["pe", "act", "dve", "pool", "sp"])

    def op(self, eng, fn, r=(), w=()):
        deps = self._deps(r, w)
        self._fence_check(eng, r, w, deps)
        self.ops.append(dict(eng=eng, fn=fn, deps=deps, dma=None))
        return len(self.ops) - 1

    def dma(self, out, in_, key, r=(), w=(), eng="sp"):
        deps = self._deps(r, w)
        self.dma_arena[key] = self._fence_check(eng, r, w, deps)
        self.dma_cnt[key] = self.dma_cnt.get(key, 0) + 16
        fn = lambda e: e.dma_start(out=out, in_=in_)
        self.ops.append(dict(eng=eng, fn=fn, deps=deps, dma=(key, self.dma_cnt[key])))
        self.dma_last[key] = len(self.ops) - 1
        return len(self.ops) - 1

    def wait_all(self, eng, idxs):
        self.ops.append(dict(eng=eng, fn=None, deps={i: True for i in idxs}, dma=None))

    def barrier(self):
        last = {}
        for i, o in enumerate(self.ops):
            if o["dma"] is None and o["fn"] is not None:
                last[o["eng"]] = i
        deps = {i: True for i in last.values()}
        for i in self.dma_last.values():
            deps[i] = True
        for eng in ("pe", "act", "dve", "pool", "sp"):
            self.ops.append(dict(eng=eng, fn=None, deps=dict(deps), dma=None, bar=True))
        self.fence_deps = {}
        self.fence_pending = set()

    def emit(self, stack):
        nc = self.nc
        ops = self.ops
        for o in ops:
            o["sig"] = False
        for o in ops:
            latest = {}
            for j, raw in o["deps"].items():
                p = ops[j]
                if p["dma"] is not None:
                    continue
                if p["eng"] == o["eng"]:
                    if o["eng"] == "pe" or o.get("bar"):
                        continue
                if j > latest.get(p["eng"], -1):
                    latest[p["eng"]] = j
            o["edeps"] = latest
            for j in latest.values():
                ops[j]["sig"] = True
        cnt = {}
        for o in ops:
            if o["dma"] is None and o["sig"]:
                cnt[o["eng"]] = cnt.get(o["eng"], 0) + 1
                o["sigval"] = cnt[o["eng"]]
        engs = ["pe", "act", "dve", "pool", "sp"]
        sems = {e: stack.enter_context(nc.semaphore("s_" + e)) for e in engs}
        dsems = {k: stack.enter_context(nc.semaphore("d_%d" % n)) for n, k in enumerate(self.dma_cnt)}
        block = stack.enter_context(nc.Block())

        def run(ename, eng):
            waited = {}
            for o in ops:
                if o["eng"] != ename:
                    continue
                wl = []
                for j, raw in o["deps"].items():
                    p = ops[j]
                    if p["dma"] is not None:
                        key = p["dma"][0]
                        wl.append((dsems[key], p["dma"][1], "d" + str(key)))
                for pe_, j in o["edeps"].items():
                    wl.append((sems[pe_], ops[j]["sigval"], pe_))
                for s, v, wk in wl:
                    if waited.get(wk, 0) >= v:
                        continue
                    waited[wk] = v
                    eng.wait_ge(s, v)
                if o["fn"] is None:
                    continue
                ins = o["fn"](eng)
                if o["dma"] is not None:
                    ins.then_inc(dsems[o["dma"][0]], 16)
                elif o["sig"]:
                    ins.then_inc(sems[ename], 1)

        @block.tensor
        def _(e):
            run("pe", e)

        @block.scalar
        def _(e):
            run("act", e)

        @block.vector
        def _(e):
            run("dve", e)

        @block.gpsimd
        def _(e):
            run("pool", e)

        @block.sync
        def _(e):
            run("sp", e)


class Res:
    def __init__(self, ap, name, names=None):
        self.ap = ap
        self.name = name
        self.names = names if names is not None else [name]


def build_nc(nseq=2, n_phases=None, layers=(0, 1)):
    nc = bass.Bass("TRN2", target_bir_lowering=False)

    def din(name, shape):
        return nc.dram_tensor(name, list(shape), F32, kind="ExternalInput").ap()

    x_d = din("x", [nseq, S, D])
    w_in_d = din("w_in", [L, D, INC])
    w_out_d = din("w_out", [L, D, D])
    fg_d = din("ffn_w_gate", [1, D, DFF])
    fu_d = din("ffn_w_up", [1, D, DFF])
    fd_d = din("ffn_w_down", [1, DFF, D])
    mg_d = din("moe_w_gate", [1, NE, D, DFE])
    mu_d = din("moe_w_up", [1, NE, D, DFE])
    md_d = din("moe_w_down", [1, NE, DFE, D])
    pcol_d = din("pcol", [128, L * NPC])
    pbc_d = din("pbc", [L, 1, 1536])
    wsT_d = din("wsT", [L, 128, 512])
    bsT_d = din("bsT", [L, 128, 256])
    routerF_d = din("routerF", [128, 8 * NE])
    wa_d = din("lru_wa", [L, 4, 64, 64])
    wx_d = din("lru_wx", [L, 4, 64, 64])
    y_d = nc.dram_tensor("y", [nseq, S, D], F32, kind="ExternalOutput").ap()

    with ExitStack() as st:
        P = Prog(nc)

        def sb(name, shape, dt=F32):
            return st.enter_context(nc.sbuf_tensor(name, shape, dt))

        X = sb("X", [128, NT, D])
        HT = sb("HT", [128, 8, S], BF16)
        YT = sb("YT", [128, 2, S], BF16)
        WS = [sb("WS%d" % i, [128, 6144], BF16) for i in range(2)]
        SCR = sb("SCR", [128, 12288])
        GNS = SCR[:, 10240:12288]
        SPR = sb("SPR", [128, 10 * 512], F32R)
        identb = sb("identb", [128, 128], BF16)
        identf = sb("identf", [128, 128])
        onesf = sb("onesf", [128, 128])
        onesb = sb("onesb", [128, 128], BF16)
        blkonesb = sb("blkonesb", [128, 128], BF16)
        ntri = sb("ntri", [128, 128], F32R)
        nones = sb("nones", [128, 128], F32R)
        MW = sb("MW", [128, 896])
        NEGW = sb("NEGW", [128, 896], BF16)
        maskle = sb("maskle", [128, 128])
        pcol = sb("pcol_s", [128, L * NPC])
        small = sb("small", [128, 512])
        PS = [st.enter_context(nc.psum_tensor("ps%d" % i, [128, 512], F32)) for i in range(8)]

        ss = small[:, 0:16]
        lnt = small[:, 16:32]
        rs = small[:, 32:48]
        qk8 = small[:, 48:52]
        clc = small[:, 52:60]
        logits = small[:, 64:192].rearrange("p (t e) -> p t e", e=NE)
        gates = small[:, 192:320].rearrange("p (t e) -> p t e", e=NE)
        tk = small[:, 320:400]
        bnst = small[:, 400:420]

        cnt = {"ps": 0, "psl": 0, "ws": 0}

        def psn():
            i = cnt["ps"] % 6
            cnt["ps"] += 1
            return Res(PS[i][:], "ps%d" % i)

        def psl():
            i = 6 + cnt["psl"] % 2
            cnt["psl"] += 1
            return Res(PS[i][:], "ps%d" % i)

        def ws_slot(i):
            return Res(WS[i][:], "WS%du" % i, ["WS%du" % i, "WS%dd" % i])

        def ws_next():
            i = cnt["ws"] % 2
            cnt["ws"] += 1
            return ws_slot(i)

        class Arena:
            def __init__(self, ap):
                self.ap = ap
                self.off = 0

            def reset(self):
                self.off = 0

            def f32(self, n):
                a = self.ap[:, self.off:self.off + n]
                self.off += n
                assert self.off <= self.ap.shape[1], self.off
                return a

            def bf16(self, n):
                assert n % 2 == 0
                return self.f32(n // 2).bitcast(BF16)

        A = Arena(SCR[:])

        def MM(out, lhsT, rhs, start, stop, r, w):
            P.op("pe", lambda e: e.matmul(out, lhsT=lhsT, rhs=rhs, start=start, stop=stop), r=r, w=w)

        def TR(out, in_, r, w):
            P.op("pe", lambda e: e.transpose(out=out, in_=in_, identity=identb[:]), r=r, w=w)

        def ACT(out, in_, func, r, w, **kw):
            P.op("act", lambda e: e.activation(out=out, in_=in_, func=func, **kw), r=r, w=w)

        def TT(out, in0, in1, op, r, w, eng="dve"):
            P.op(eng, lambda e: e.tensor_tensor(out=out, in0=in0, in1=in1, op=op), r=r, w=w)

        def TS(out, in0, s1, s2, op0, op1, r, w, eng="dve"):
            if s2 is None:
                P.op(eng, lambda e: e.tensor_scalar(out=out, in0=in0, scalar1=s1, scalar2=None, op0=op0), r=r, w=w)
            else:
                P.op(eng, lambda e: e.tensor_scalar(out=out, in0=in0, scalar1=s1, scalar2=s2, op0=op0, op1=op1), r=r, w=w)

        def STT(out, in0, scalar, in1, op0, op1, r, w, **kw):
            P.op("dve", lambda e: e.scalar_tensor_tensor(out=out, in0=in0, scalar=scalar, in1=in1, op0=op0, op1=op1, **kw), r=r, w=w)

        def CP(out, in_, r, w, eng="dve"):
            P.op(eng, lambda e: e.tensor_copy(out=out, in_=in_), r=r, w=w)

        def MS(ap, val, w, eng="dve"):
            P.op(eng, lambda e: e.memset(ap, val), w=w)

        def pc(l, col, n=1):
            return pcol[:, l * NPC + col: l * NPC + col + n]

        io = SCR[:, 0:896].bitcast(I32)
        iof = SCR[:, 896:1792]
        P.op("pool", lambda e: e.iota(io, pattern=[[1, 896]], base=0, channel_multiplier=-1), w=["io"])
        CP(iof, io, r=["io"], w=["iof"])
        P.op("dve", lambda e: e.tensor_single_scalar(out=MW[:], in_=iof, scalar=384.0, op=ALU.is_gt), r=["iof"], w=["cMW"])
        P.op("dve", lambda e: e.tensor_scalar(out=NEGW[:], in0=MW[:], scalar1=30000.0, scalar2=-30000.0, op0=ALU.mult, op1=ALU.add), r=["cMW"], w=["c"])
        P.op("dve", lambda e: e.tensor_single_scalar(out=identf[:], in_=iof[:, 0:128], scalar=0.0, op=ALU.is_equal), r=["iof"], w=["c"])
        P.op("dve", lambda e: e.tensor_single_scalar(out=identb[:], in_=iof[:, 0:128], scalar=0.0, op=ALU.is_equal), r=["iof"], w=["c"])
        P.op("dve", lambda e: e.tensor_single_scalar(out=maskle[:], in_=iof[:, 0:128], scalar=0.0, op=ALU.is_ge), r=["iof"], w=["c"])
        P.op("dve", lambda e: e.tensor_scalar(out=ntri[:], in0=iof[:, 0:128], scalar1=0.0, scalar2=-1.0, op0=ALU.is_le, op1=ALU.mult), r=["iof"], w=["c"])
        MS(onesf[:], 1.0, w=["c"])
        MS(onesb[:], 1.0, w=["c"])
        P.op("dve", lambda e: e.tensor_scalar(out=nones[:], in0=iof[:, 0:128], scalar1=0.0, scalar2=-1.0, op0=ALU.mult, op1=ALU.add), r=["iof"], w=["c"])
        MS(blkonesb[:], 0.0, w=["c"])
        MS(blkonesb[0:64, 0:64], 1.0, w=["c"])
        MS(blkonesb[64:128, 64:128], 1.0, w=["c"])
        P.dma(pcol[:], pcol_d, "pcol", w=["pcol"])
        for l in range(L):
            TS(qk8[:, l:l + 1], pc(l, 38), 0.125, None, ALU.mult, None, r=["pcol"], w=["c"])
        P.barrier()

        def phase_load(sq):
            yield
            xs = x_d[sq].rearrange("(t p) d -> p t d", p=128)
            for g in range(4):
                P.dma(X[:, 4 * g:4 * g + 4, :], xs[:, 4 * g:4 * g + 4, :], "xin%d" % g, w=["X%d" % t for t in range(4 * g, 4 * g + 4)])

        stores = []

        def phase_store(sq):
            yield
            ys = y_d[sq].rearrange("(t p) d -> p t d", p=128)
            for g in range(4):
                stores.append(P.dma(ys[:, 4 * g:4 * g + 4, :], X[:, 4 * g:4 * g + 4, :], "xout%d" % g, r=["X%d" % t for t in range(4 * g, 4 * g + 4)]))

        def phase_norm(l, gcol0):
            yield
            A.reset()
            hss = [A.bf16(4096).rearrange("p (i d) -> p i d", i=4) for _ in range(2)]
            junk = A.bf16(1024)

            def N1(g):
                hs = hss[g % 2]
                for i in range(4):
                    t = 4 * g + i
                    ACT(junk, X[:, t, :], AF.Square, r=["X%d" % t], w=["junk", "ss"], accum_out=ss[:, t:t + 1])
                ACT(lnt[:, 4 * g:4 * g + 4], ss[:, 4 * g:4 * g + 4], AF.Ln, r=["ss"], w=["lnt"], scale=1.0 / D, bias=EPS)
                ACT(rs[:, 4 * g:4 * g + 4], lnt[:, 4 * g:4 * g + 4], AF.Exp, r=["lnt"], w=["rs"], scale=-0.5)
                for i in range(4):
                    t = 4 * g + i
                    TS(hs[:, i, :], X[:, t, :], rs[:, t:t + 1], None, ALU.mult, None, r=["X%d" % t, "rs"], w=["hs%d_%d" % (g % 2, i)])

            def N2(g):
                hs = hss[g % 2]
                for c in range(8):
                    pb = psn()
                    ptb = pb.ap.bitcast(BF16)
                    for i in range(4):
                        TR(ptb[:, 128 * i:128 * i + 128], hs[:, i, 128 * c:128 * c + 128], r=["hs%d_%d" % (g % 2, i)], w=[pb.name])
                    dst = HT[:, c, 512 * g:512 * g + 512]
                    if c % 2 == 0:
                        ACT(dst, ptb[:, 0:512], AF.Identity, r=[pb.name], w=["HT%d" % g], scale=pc(l, gcol0 + c))
                    else:
                        TS(dst, ptb[:, 0:512], pc(l, gcol0 + c), None, ALU.mult, None, r=[pb.name], w=["HT%d" % g])

            N1(0)
            for g in range(4):
                if g + 1 < 4:
                    N1(g + 1)
                N2(g)

        def w_in_view(l):
            return w_in_d[l].rearrange("(c p) n -> p c n", p=128)

        def group_norm_out(l, gi):
            slot = ws_next()
            w3 = slot.ap[:, 0:2048].rearrange("p (c n) -> p c n", c=2)
            P.dma(w3, w_out_d[l][256 * gi:256 * gi + 256, :].rearrange("(c p) n -> p c n", p=128), slot.name,
                  w=slot.names, eng="pool")
            sqb = [GNS[:, 256 * i:256 * i + 256].bitcast(BF16) for i in range(4)]
            lrs = [GNS[:, 1024:1536], GNS[:, 1536:2048]]
            gheld = {}

            def GP(g):
                sl = slice(512 * g, 512 * g + 512)
                sq0 = sqb[2 * (g % 2)]
                sq1 = sqb[2 * (g % 2) + 1]
                n0 = "gsq%d" % (2 * (g % 2))
                n1 = "gsq%d" % (2 * (g % 2) + 1)
                ACT(sq0, YT[:, 0, sl], AF.Square, r=["YT0_%d" % g], w=[n0])
                ACT(sq1, YT[:, 1, sl], AF.Square, r=["YT1_%d" % g], w=[n1])
                pb = psn()
                MM(pb.ap, onesb[:], sq0, True, False, r=[n0], w=[pb.name])
                MM(pb.ap, onesb[:], sq1, False, True, r=[n1], w=[pb.name])
                gheld[g] = pb

            def GQ(g):
                sl = slice(512 * g, 512 * g + 512)
                pb = gheld.pop(g)
                rr = lrs[g % 2]
                rn = "grr%d" % (g % 2)
                ACT(rr, pb.ap, AF.Ln, r=[pb.name], w=[rn], scale=1.0 / 256, bias=EPS)
                ACT(rr, rr, AF.Exp, r=[rn], w=[rn], scale=-0.5)
                for c in range(2):
                    STT(YT[:, c, sl], YT[:, c, sl], pc(l, 16 + 2 * gi + c), rr, ALU.mult, ALU.mult,
                        r=["YT%d_%d" % (c, g), rn], w=["YT%d_%d" % (c, g)])

            GP(0)
            for g in range(4):
                if g + 1 < 4:
                    GP(g + 1)
                GQ(g)
            for g in range(4):
                for i in range(4):
                    t = 4 * g + i
                    for h in range(2):
                        py = psn()
                        for c in range(2):
                            MM(py.ap, YT[:, c, 128 * t:128 * t + 128], w3[:, c, 512 * h:512 * h + 512], c == 0, c == 1,
                               r=["YT%d_%d" % (c, g)] + slot.names, w=[py.name])
                        xs_ = X[:, t, 512 * h:512 * h + 512]
                        TT(xs_, py.ap, xs_, ALU.add, r=[py.name, "X%d" % t], w=["X%d" % t])

        def phase_attn(l, pr):
            slot = ws_next()
            wv3 = slot.ap[:, 0:3072].rearrange("p (c n) -> p c n", c=8)
            for j, c0 in enumerate([128 * pr, 256 + 128 * pr, 512 + 128 * pr]):
                P.dma(wv3[:, :, 128 * j:128 * j + 128], w_in_view(l)[:, :, c0:c0 + 128], slot.name, w=slot.names, eng="pool")
            yield
            A.reset()
            QTh = [A.bf16(2048) for _ in range(2)]
            KT = A.bf16(2048)
            Vpf = A.bf16(4096)
            Vp = Vpf.rearrange("p (s h n) -> p s h n", s=16, h=2)
            eb = [A.f32(512) for _ in range(5)]
            spb = [SPR[:, 512 * i:512 * i + 512].bitcast(F32) for i in range(5)]
            Rb = [SPR[:, 512 * i:512 * i + 512].bitcast(F32) for i in range(5, 10)]
            wb = [A.bf16(512) for _ in range(4)]
            sqs = [A.bf16(512) for _ in range(2)]
            rrs = [A.f32(512) for _ in range(2)]
            assert A.off <= 10240
            MS(Vpf, 0.0, w=["Vp%d" % g for g in range(4)])
            MS(QTh[0][64:128, :], 0.0, w=["QTz"])
            MS(QTh[1][0:64, :], 0.0, w=["QTz"])
            items = [(g, wi) for g in range(4) for wi in range(2)]
            held = {}

            def PJ(i):
                g, wi = items[i]
                sl = slice(512 * g, 512 * g + 512)
                pa = psn()
                for dc in range(8):
                    MM(pa.ap, wv3[:, dc, 128 * wi:128 * wi + 128], HT[:, dc, sl], dc == 0, dc == 7,
                       r=slot.names + ["HT%d" % g], w=[pa.name])
                sq_ = sqs[i % 2]
                sqn = "asq%d" % (i % 2)
                ACT(sq_, pa.ap, AF.Square, r=[pa.name], w=[sqn])
                pk = psn()
                MM(pk.ap, blkonesb[:], sq_, True, True, r=[sqn], w=[pk.name])
                held[i] = (pa, pk)

            def QJ(i):
                g, wi = items[i]
                sl = slice(512 * g, 512 * g + 512)
                pa, pk = held.pop(i)
                rr = rrs[i % 2]
                rn = "arr%d" % (i % 2)
                ACT(rr, pk.ap, AF.Ln, r=[pk.name], w=[rn], scale=1.0 / 64, bias=EPS)
                ACT(rr, rr, AF.Exp, r=[rn], w=[rn], scale=-0.5)
                if wi == 0:
                    for h in range(2):
                        hp = slice(64 * h, 64 * h + 64)
                        STT(QTh[h][hp, sl], pa.ap[hp, :], qk8[hp, l:l + 1], rr[hp, :], ALU.mult, ALU.mult,
                            r=[pa.name, rn, "QTz"], w=["QT%d" % g])
                else:
                    STT(KT[:, sl], pa.ap, pc(l, 39), rr, ALU.mult, ALU.mult, r=[pa.name, rn], w=["KT%d" % g])

            def VJ(g):
                pv = psn()
                for i in range(4):
                    t = 4 * g + i
                    for dc in range(8):
                        MM(pv.ap[:, 128 * i:128 * i + 128], HT[:, dc, 128 * t:128 * t + 128], wv3[:, dc, 256:384],
                           dc == 0, dc == 7, r=slot.names + ["HT%d" % g], w=[pv.name])
                pv3 = pv.ap.rearrange("p (i n) -> p i n", i=4)
                ACT(Vp[:, 4 * g:4 * g + 4, 0, 0:64], pv3[:, :, 0:64], AF.Copy, r=[pv.name], w=["Vp%d" % g])
                CP(Vp[:, 4 * g:4 * g + 4, 1, 64:128], pv3[:, :, 64:128], r=[pv.name], w=["Vp%d" % g])

            PJ(0)
            for i in range(len(items)):
                if i + 1 < len(items):
                    PJ(i + 1)
                QJ(i)
                if i % 2 == 1:
                    VJ(i // 2)
            blocks = []
            for qt in range(4):
                for h in range(2):
                    jmax = 4 * qt + 3
                    for j in range(jmax, -1, -1):
                        blocks.append(dict(qt=qt, h=h, j=j, k=j - 4 * qt, pos=jmax - j,
                                           first=(h == 0 and j == jmax), last=(h == 1 and j == 0)))
            for n, b in enumerate(blocks):
                b["n"] = n
            pos = {}

            def zmm(pt, b, stop_):
                qs = slice(512 * b["qt"], 512 * b["qt"] + 512)
                ks = slice(128 * b["j"], 128 * b["j"] + 128)
                rq = ["KT%d" % (b["j"] // 4), "QT%d" % b["qt"]]
                k = b["k"]
                MM(pt.ap, KT[:, ks], QTh[b["h"]][:, qs], True, stop_ and k < 0, r=rq, w=[pt.name])
                if k >= 0:
                    MM(pt.ap, identb[:], NEGW[:, 384 - 128 * k:384 - 128 * k + 512], False, stop_, r=[], w=[pt.name])

            def S1(b):
                n = b["n"]
                pz = psn()
                zmm(pz, b, True)
                e_ = eb[n % 5]
                en = "ae%d" % (n % 5)
                ACT(e_, pz.ap, AF.Exp, r=[pz.name], w=[en])
                sp = spb[n % 5]
                spn = "sp%d" % (n % 5)
                ACT(sp.bitcast(F32R), e_, AF.Ln, r=[en], w=[spn], bias=1.0)
                if b["pos"] == 0:
                    b["R"] = None
                else:
                    prev = blocks[n - 1]
                    psp = spb[(n - 1) % 5]
                    pspn = "sp%d" % ((n - 1) % 5)
                    nr = Rb[n % 5]
                    nrn = "aR%d" % (n % 5)
                    if prev["R"] is None:
                        P.op("dve", lambda e: e.tensor_copy(out=nr.bitcast(F32R), in_=psp), r=[pspn], w=[nrn])
                    else:
                        TT(nr.bitcast(F32R), prev["R"].ap, psp, ALU.add, r=[prev["R"].name, pspn], w=[nrn])
                    b["R"] = Res(nr, nrn)

            def S2(b):
                n = b["n"]
                sp = spb[n % 5]
                spn = "sp%d" % (n % 5)
                R = b["R"]
                pb = psn()
                zmm(pb, b, False)
                MM(pb.ap, ntri[:], sp.bitcast(F32R), False, R is None, r=[spn], w=[pb.name])
                if R is not None:
                    MM(pb.ap, nones[:], R.ap.bitcast(F32R), False, True, r=[R.name], w=[pb.name])
                w_ = wb[n % 4]
                wn = "aw%d" % (n % 4)
                ACT(w_, pb.ap, AF.Exp, r=[pb.name], w=[wn])

            def S3(b):
                n = b["n"]
                qs = slice(512 * b["qt"], 512 * b["qt"] + 512)
                if b["first"]:
                    pos["po"] = psl()
                po = pos["po"]
                w_ = wb[n % 4]
                wn = "aw%d" % (n % 4)
                MM(po.ap, Vp[:, b["j"], b["h"], :], w_, b["first"], b["last"], r=["Vp%d" % (b["j"] // 4), wn], w=[po.name])
                if b["last"]:
                    ACT(YT[:, pr, qs], po.ap, AF.Copy, r=[po.name], w=["YT%d_%d" % (pr, b["qt"])])

            LOOK = 2
            for i in range(len(blocks) + 2 * LOOK):
                if i < len(blocks):
                    S1(blocks[i])
                if 0 <= i - LOOK < len(blocks):
                    S2(blocks[i - LOOK])
                if 0 <= i - 2 * LOOK < len(blocks):
                    S3(blocks[i - 2 * LOOK])
            if pr == 1:
                P.fence()
                group_norm_out(l, 0)

        def phase_conf(l):
            slot = ws_next()
            wv3 = slot.ap[:, 0:4096].rearrange("p (c n) -> p c n", c=8)
            P.dma(wv3, w_in_view(l)[:, :, 768:1280], slot.name, w=slot.names, eng="pool")
            yield
            A.reset()
            hpad = A.bf16(4160).rearrange("p (c n) -> p c n", c=2)
            dg = A.bf16(7936).rearrange("p (c k n) -> p c k n", c=2, k=31)
            sig = A.f32(512)
            cv = A.f32(1024).rearrange("p (c n) -> p c n", c=2)
            sqv = A.bf16(1024).rearrange("p (c n) -> p c n", c=2)
            mean = A.f32(512)
            tmp = A.f32(512)
            rr = A.f32(512)
            for c in range(2):
                MS(hpad[:, c, 0:30], 0.0, w=["hp%d" % c])
                for k in range(31):
                    TS(dg[:, c, k, :], identf[:], pc(l, 40 + 31 * c + k), None, ALU.mult, None, r=[], w=["dg"])
            for g in range(4):
                sl = slice(512 * g, 512 * g + 512)
                for c in range(2):
                    pv = psn()
                    pg = psn()
                    for dc in range(8):
                        MM(pv.ap, wv3[:, dc, 128 * c:128 * c + 128], HT[:, dc, sl], dc == 0, dc == 7, r=slot.names + ["HT%d" % g], w=[pv.name])
                    for dc in range(8):
                        MM(pg.ap, wv3[:, dc, 256 + 128 * c:256 + 128 * c + 128], HT[:, dc, sl], dc == 0, dc == 7, r=slot.names + ["HT%d" % g], w=[pg.name])
                    ACT(sig, pg.ap, AF.Sigmoid, r=[pg.name], w=["csig"])
                    TT(hpad[:, c, 30 + 512 * g:30 + 512 * g + 512], pv.ap, sig, ALU.mult, r=[pv.name, "csig"], w=["hp%d" % c])
            for g in range(4):
                sl = slice(512 * g, 512 * g + 512)
                for c in range(2):
                    pcv = psn()
                    for k in range(31):
                        MM(pcv.ap, dg[:, c, k, :], hpad[:, c, 512 * g + k:512 * g + k + 512], k == 0, k == 30, r=["dg", "hp%d" % c], w=[pcv.name])
                    ACT(cv[:, c, :], pcv.ap, AF.Identity, r=[pcv.name], w=["cv%d" % c], bias=pc(l, 24 + c))
                    ACT(sqv[:, c, :], cv[:, c, :], AF.Square, r=["cv%d" % c], w=["csq%d" % c])
                p1 = psn()
                p2 = psn()
                for c in range(2):
                    MM(p1.ap, onesf[:], cv[:, c, :], c == 0, c == 1, r=["cv%d" % c], w=[p1.name])
                for c in range(2):
                    MM(p2.ap, onesb[:], sqv[:, c, :], c == 0, c == 1, r=["csq%d" % c], w=[p2.name])
                TS(mean, p1.ap, 1.0 / 256, None, ALU.mult, None, r=[p1.name], w=["cmean"])
                TT(tmp, mean, mean, ALU.mult, r=["cmean"], w=["ctmp"])
                STT(tmp, p2.ap, 1.0 / 256, tmp, ALU.mult, ALU.subtract, r=[p2.name, "ctmp"], w=["ctmp"])
                ACT(tmp, tmp, AF.Ln, r=["ctmp"], w=["ctmp"], bias=EPS)
                ACT(rr, tmp, AF.Exp, r=["ctmp"], w=["crr"], scale=-0.5)
                for c in range(2):
                    TT(cv[:, c, :], cv[:, c, :], mean, ALU.subtract, r=["cv%d" % c, "cmean"], w=["cv%d" % c])
                    TT(cv[:, c, :], cv[:, c, :], rr, ALU.mult, r=["cv%d" % c, "crr"], w=["cv%d" % c])
                    ACT(YT[:, c, sl], cv[:, c, :], AF.Silu, r=["cv%d" % c], w=["YT%d_%d" % (c, g)],
                        scale=pc(l, 26 + c), bias=pc(l, 28 + c))
            group_norm_out(l, 1)

        def phase_gmlp(l):
            slot = ws_next()
            wv3 = slot.ap[:, 0:4096].rearrange("p (c n) -> p c n", c=8)
            P.dma(wv3, w_in_view(l)[:, :, 1280:1792], slot.name, w=slot.names, eng="pool")
            yield
            A.reset()
            uT = A.bf16(4096).rearrange("p (c n) -> p c n", c=2)
            vpf = A.bf16(8192)
            vp = vpf.rearrange("p (s h n) -> p s h n", s=16, h=4)
            WTr = A.bf16(512).rearrange("p (h t) -> p h t", h=4)
            WTm = A.bf16(512).rearrange("p (h t) -> p h t", h=4)
            bsT = A.f32(256).rearrange("p (c t) -> p c t", c=2)
            bs4 = A.f32(1024).rearrange("p (c q t) -> p c q t", c=2, q=4)
            gb = A.f32(512)
            gvb = [A.f32(256) for _ in range(2)]
            vnb = [A.f32(256) for _ in range(2)]
            tmp = A.f32(512)
            P.dma(WTr, wsT_d[l].rearrange("p (h t) -> p h t", h=4), "gWT", w=["gWTr"], eng="pool")
            P.dma(bsT, bsT_d[l].rearrange("p (c t) -> p c t", c=2), "gbs", w=["gbsT"])
            P.dma(gb, pbc_d[l][:, 0:512].partition_broadcast(128), "ggb", w=["ggb"])
            MS(vpf, 0.0, w=["gvp"])
            for h in range(4):
                TT(WTm[:, h, :], WTr[:, h, :], maskle[:], ALU.mult, r=["gWTr"], w=["gWTm"])
            for c in range(2):
                for q in range(4):
                    CP(bs4[:, c, q, :], bsT[:, c, :], r=["gbsT"], w=["gbs4"])
            for g in range(4):
                sl = slice(512 * g, 512 * g + 512)
                for c in range(2):
                    pu = psn()
                    for dc in range(8):
                        MM(pu.ap, wv3[:, dc, 128 * c:128 * c + 128], HT[:, dc, sl], dc == 0, dc == 7, r=slot.names + ["HT%d" % g], w=[pu.name])
                    ACT(uT[:, c, sl], pu.ap, AF.Gelu, r=[pu.name], w=["guT%d" % g])
            mv = small[:, 420:452].rearrange("p (t k) -> p t k", k=2)
            lnb = small[:, 452:468]
            rsb = small[:, 468:484]
            for t in range(NT):
                g = t // 4
                pv = psn()
                for dc in range(8):
                    MM(pv.ap[:, 0:256], HT[:, dc, 128 * t:128 * t + 128], wv3[:, dc, 256:512], dc == 0, dc == 7, r=slot.names + ["HT%d" % g], w=[pv.name])
                gv = gvb[t % 2]
                gvn = "ggv%d" % (t % 2)
                ACT(gv, pv.ap[:, 0:256], AF.Gelu, r=[pv.name], w=[gvn])
                P.op("dve", lambda e, gv=gv: e.bn_stats(out=bnst[:, 0:6], in_=gv), r=[gvn], w=["gbn6"])
                P.op("dve", lambda e, t=t: e.bn_aggr(out=mv[:, t, :], in_=bnst[:, 0:6]), r=["gbn6"], w=["gmv"])
                gv3 = gv.rearrange("p (h d) -> p h d", d=64)
                for hh in range(2):
                    CP(vp[:, t, hh::2, 64 * hh:64 * hh + 64], gv3[:, hh::2, :], r=[gvn, "gvp"], w=["gvp%d" % t])
            ACT(lnb, mv[:, :, 1], AF.Ln, r=["gmv"], w=["glnb"], bias=EPS)
            ACT(rsb, lnb, AF.Exp, r=["glnb"], w=["grsb"], scale=-0.5)
            g3 = gb[:, 0:256].rearrange("p (h d) -> p h d", d=64)
            b3 = gb[:, 256:512].rearrange("p (h d) -> p h d", d=64)
            for t in range(NT):
                for hh in range(2):
                    v_ = vp[:, t, hh::2, 64 * hh:64 * hh + 64]
                    TS(v_, v_, mv[:, t, 0:1], rsb[:, t:t + 1], ALU.subtract, ALU.mult, r=["gvp%d" % t, "gmv", "grsb"], w=["gvp%d" % t])
                    TT(v_, v_, g3[:, hh::2, :], ALU.mult, r=["gvp%d" % t, "ggb"], w=["gvp%d" % t])
                    TT(v_, v_, b3[:, hh::2, :], ALU.add, r=["gvp%d" % t, "ggb"], w=["gvp%d" % t])
            for g in range(4):
                sl = slice(512 * g, 512 * g + 512)
                for c in range(2):
                    pm = psn()
                    for q in range(4):
                        cb = 4 * g + q
                        for hh in range(2):
                            h = 2 * c + hh
                            MM(pm.ap[:, 128 * q:128 * q + 128], vp[:, cb, h, :], WTm[:, h, :], hh == 0, hh == 1, r=["gvp%d" % cb, "gWTm"], w=[pm.name])
                    TT(tmp, pm.ap, bs4[:, c, :, :].rearrange("p q t -> p (q t)"), ALU.add, r=[pm.name, "gbs4"], w=["gtmp"])
                    TT(YT[:, c, sl], tmp, uT[:, c, sl], ALU.mult, r=["gtmp", "guT%d" % g], w=["YT%d_%d" % (c, g)])
            group_norm_out(l, 2)

        def phase_lru(l):
            slot = ws_next()
            wv3 = slot.ap[:, 0:4096].rearrange("p (c n) -> p c n", c=8)
            P.dma(wv3, w_in_view(l)[:, :, 1792:2304], slot.name, w=slot.names, eng="pool")
            yield
            A.reset()
            xpad = A.f32(2052)
            xb = A.f32(2048)
            xbb = A.bf16(2048)
            gl = A.bf16(2048)
            bdf = A.bf16(512)
            bd = bdf.rearrange("p (a c n) -> p a c n", a=2, c=2)
            hq = [A.f32(512) for _ in range(2)]
            r_ = A.f32(512)
            i_ = A.f32(512)
            a_ = A.f32(512)
            s_ = A.f32(512)
            b_ = A.f32(512)
            MS(bdf, 0.0, w=["lbd"])
            for a, src in enumerate([wa_d, wx_d]):
                for c in range(2):
                    for hh in range(2):
                        P.dma(bd[64 * hh:64 * hh + 64, a, c, 64 * hh:64 * hh + 64], src[l, 2 * c + hh], "lbd", r=[], w=["lbd"], eng="pool")
            ACT(clc[:, 4:6], pc(l, 36, 2), AF.Exp, r=[], w=["lcl_t"], scale=-1.0)
            ACT(clc[:, 6:8], clc[:, 4:6], AF.Ln, r=["lcl_t"], w=["lcl_u"], bias=1.0)
            TS(clc[:, 0:2], clc[:, 6:8], -8.0, None, ALU.mult, None, r=["lcl_u"], w=["lcl"])
            TS(clc[:, 2:4], clc[:, 6:8], -16.0, None, ALU.mult, None, r=["lcl_u"], w=["lcl"])
            for c in range(2):
                MS(xpad[:, 0:3], 0.0, w=["lxp"])
                for g in range(4):
                    sl = slice(512 * g, 512 * g + 512)
                    px = psn()
                    pg = psn()
                    for dc in range(8):
                        MM(px.ap, wv3[:, dc, 128 * c:128 * c + 128], HT[:, dc, sl], dc == 0, dc == 7, r=slot.names + ["HT%d" % g], w=[px.name])
                    for dc in range(8):
                        MM(pg.ap, wv3[:, dc, 256 + 128 * c:256 + 128 * c + 128], HT[:, dc, sl], dc == 0, dc == 7, r=slot.names + ["HT%d" % g], w=[pg.name])
                    ACT(xpad[:, 3 + 512 * g:3 + 512 * g + 512], px.ap, AF.Copy, r=[px.name], w=["lxp"])
                    ACT(gl[:, sl], pg.ap, AF.Gelu, r=[pg.name], w=["lgl"])
                TS(xb, xpad[:, 0:2048], pc(l, 102 + 4 * c), pc(l, 30 + c), ALU.mult, ALU.add, r=["lxp"], w=["lxb"])
                for k in range(1, 4):
                    STT(xb, xpad[:, k:k + 2048], pc(l, 102 + 4 * c + k), xb, ALU.mult, ALU.add, r=["lxp", "lxb"], w=["lxb"])
                ACT(xbb, xb, AF.Copy, r=["lxb"], w=["lxbb"])
                for g in range(4):
                    sl = slice(512 * g, 512 * g + 512)
                    pr_ = psn()
                    pi_ = psn()
                    MM(pr_.ap, bd[:, 0, c, :], xbb[:, sl], True, True, r=["lbd", "lxbb"], w=[pr_.name])
                    MM(pi_.ap, bd[:, 1, c, :], xbb[:, sl], True, True, r=["lbd", "lxbb"], w=[pi_.name])
                    ACT(r_, pr_.ap, AF.Sigmoid, r=[pr_.name], w=["lr"], bias=pc(l, 32 + c))
                    ACT(i_, pi_.ap, AF.Sigmoid, r=[pi_.name], w=["li"], bias=pc(l, 34 + c))
                    ACT(a_, r_, AF.Exp, r=["lr", "lcl"], w=["la"], scale=clc[:, c:c + 1])
                    ACT(s_, r_, AF.Exp, r=["lr", "lcl"], w=["ls"], scale=clc[:, 2 + c:3 + c])
                    ACT(s_, s_, AF.Sqrt, r=["ls"], w=["ls"], scale=-1.0, bias=1.0)
                    TT(b_, i_, xb[:, sl], ALU.mult, r=["li", "lxb"], w=["lb"])
                    TT(b_, b_, s_, ALU.mult, r=["lb", "ls"], w=["lb"])
                    hcur = hq[g % 2]
                    hprev = hq[(g + 1) % 2]
                    init = 0.0 if g == 0 else hprev[:, 511:512]
                    rd = ["la", "lb"] + ([] if g == 0 else ["lh%d" % ((g + 1) % 2)])
                    P.op("dve", lambda e, hcur=hcur, init=init: e.tensor_tensor_scan(out=hcur, data0=a_, data1=b_, initial=init, op0=ALU.mult, op1=ALU.add),
                         r=rd, w=["lh%d" % (g % 2)])
                    TT(YT[:, c, sl], hcur, gl[:, sl], ALU.mult, r=["lh%d" % (g % 2), "lgl"], w=["YT%d_%d" % (c, g)])
            group_norm_out(l, 3)

        def phase_router(l):
            yield
            A.reset()
            rfm = A.f32(64).rearrange("p (c e) -> p c e", c=8)
            Rg = A.f32(64).rearrange("p (c e) -> p c e", c=8)
            hn = [A.f32(1024) for _ in range(2)]
            hT = [A.f32(1024) for _ in range(2)]
            P.dma(rfm, routerF_d.rearrange("p (c e) -> p c e", c=8), "rtB", w=["rfm"])
            for dc in range(8):
                TS(Rg[:, dc, :], rfm[:, dc, :], pc(l, 8 + dc), None, ALU.mult, None, r=["rfm"], w=["Rg"])
            for t in range(NT):
                h_ = hn[t % 2]
                hnn = "rhn%d" % (t % 2)
                hT_ = hT[t % 2]
                TS(h_, X[:, t, :], rs[:, t:t + 1], None, ALU.mult, None, r=["X%d" % t, "rs"], w=[hnn])
                for half in range(2):
                    pb = psn()
                    for i in range(4):
                        dc = 4 * half + i
                        P.op("pe", lambda e, o=pb.ap[:, 128 * i:128 * i + 128], a=h_[:, 128 * dc:128 * dc + 128]:
                             e.transpose(out=o, in_=a, identity=identf[:]), r=[hnn], w=[pb.name])
                    htn = "rhT%d_%d" % (t % 2, half)
                    if half == 0:
                        ACT(hT_[:, 0:512], pb.ap, AF.Copy, r=[pb.name], w=[htn])
                    else:
                        CP(hT_[:, 512:1024], pb.ap, r=[pb.name], w=[htn])
                pl = psn()
                for dc in range(8):
                    MM(pl.ap[:, 0:8], hT_[:, 128 * dc:128 * dc + 128], Rg[:, dc, :], dc == 0, dc == 7,
                       r=["rhT%d_%d" % (t % 2, dc // 4), "Rg"], w=[pl.name])
                CP(logits[:, t, :], pl.ap[:, 0:8], r=[pl.name], w=["rlg%d" % t])
                lg = logits[:, t, :]
                m1 = tk[:, 0:1]
                m2 = tk[:, 1:2]
                dd = tk[:, 2:3]
                g1 = tk[:, 3:4]
                g2 = tk[:, 4:5]
                eq1 = tk[:, 8:16]
                eq2 = tk[:, 16:24]
                l2 = tk[:, 24:32]
                P.op("dve", lambda e, lg=lg: e.reduce_max(out=m1, in_=lg, axis=AX.X), r=["rlg%d" % t], w=["rm1"])
                TS(eq1, lg, m1, None, ALU.is_equal, None, r=["rlg%d" % t, "rm1"], w=["req1"])
                STT(l2, eq1, -1e30, lg, ALU.mult, ALU.add, r=["req1", "rlg%d" % t], w=["rl2"])
                P.op("dve", lambda e: e.reduce_max(out=m2, in_=l2, axis=AX.X), r=["rl2"], w=["rm2"])
                TS(eq2, l2, m2, None, ALU.is_equal, None, r=["rl2", "rm2"], w=["req2"])
                TT(dd, m1, m2, ALU.subtract, r=["rm1", "rm2"], w=["rdd"])
                ACT(g1, dd, AF.Sigmoid, r=["rdd"], w=["rg1"])
                ACT(g2, dd, AF.Sigmoid, r=["rdd"], w=["rgg2"], scale=-1.0)
                TS(gates[:, t, :], eq1, g1, None, ALU.mult, None, r=["req1", "rg1"], w=["gates"])
                STT(gates[:, t, :], eq2, g2, gates[:, t, :], ALU.mult, ALU.add, r=["req2", "rgg2", "gates"], w=["gates"])

        def phase_ffn(l, moe):
            if moe:
                groups = [(e, f) for e in range(NE) for f in range(DFE // 256)]
            else:
                groups = [(0, f) for f in range(DFF // 256)]

            def load_up(n, slot):
                e, f = groups[n]
                gsrc = mg_d[0, e] if moe else fg_d[0]
                usrc = mu_d[0, e] if moe else fu_d[0]
                for j, src in enumerate([gsrc, usrc]):
                    P.dma(slot.ap[:, 2048 * j:2048 * j + 2048].rearrange("p (c n) -> p c n", c=8),
                          src.rearrange("(c p) n -> p c n", p=128)[:, :, 256 * f:256 * f + 256],
                          slot.names[0], w=[slot.names[0]], eng="pool")

            def load_dn(n, slot):
                e, f = groups[n]
                dsrc = md_d[0, e] if moe else fd_d[0]
                P.dma(slot.ap[:, 4096:6144].rearrange("p (c n) -> p c n", c=2),
                      dsrc[256 * f:256 * f + 256, :].rearrange("(c p) n -> p c n", p=128),
                      slot.names[1], w=[slot.names[1]], eng="pool")

            base = cnt["ws"]
            cnt["ws"] += len(groups)

            def slot_of(n):
                return ws_slot((base + n) % 2)

            s0 = slot_of(0)
            load_up(0, s0)
            load_dn(0, s0)
            yield
            A.reset()
            AT = [A.bf16(4096).rearrange("p (c n) -> p c n", c=2) for _ in range(2)]
            sgb = [A.f32(512) for _ in range(2)]
            if len(groups) > 1:
                s1 = slot_of(1)
                load_up(1, s1)
                load_dn(1, s1)
            k = [0]

            def up(n):
                slot = slot_of(n)
                at = AT[n % 2]
                wg3 = slot.ap[:, 0:2048].rearrange("p (c n) -> p c n", c=8)
                wu3 = slot.ap[:, 2048:4096].rearrange("p (c n) -> p c n", c=8)
                for g in range(4):
                    sl = slice(512 * g, 512 * g + 512)
                    for fc in range(2):
                        pg = psn()
                        pu = psn()
                        for dc in range(8):
                            MM(pg.ap, wg3[:, dc, 128 * fc:128 * fc + 128], HT[:, dc, sl], dc == 0, dc == 7, r=[slot.names[0], "HT%d" % g], w=[pg.name])
                        for dc in range(8):
                            MM(pu.ap, wu3[:, dc, 128 * fc:128 * fc + 128], HT[:, dc, sl], dc == 0, dc == 7, r=[slot.names[0], "HT%d" % g], w=[pu.name])
                        sg = sgb[k[0] % 2]
                        sgn = "fsg%d" % (k[0] % 2)
                        k[0] += 1
                        ACT(sg, pg.ap, AF.Silu, r=[pg.name], w=[sgn])
                        TT(at[:, fc, sl], sg, pu.ap, ALU.mult, r=[sgn, pu.name], w=["fAT%d" % (n % 2)])
                        yield

            def down(n):
                e, f = groups[n]
                slot = slot_of(n)
                at = AT[n % 2]
                wd3 = slot.ap[:, 4096:6144].rearrange("p (c n) -> p c n", c=2)
                for t in range(NT):
                    for h in range(2):
                        py = psn()
                        for fc in range(2):
                            MM(py.ap, at[:, fc, 128 * t:128 * t + 128], wd3[:, fc, 512 * h:512 * h + 512], fc == 0, fc == 1,
                               r=["fAT%d" % (n % 2), slot.names[1]], w=[py.name])
                        xs_ = X[:, t, 512 * h:512 * h + 512]
                        if moe:
                            STT(xs_, py.ap, gates[:, t, e:e + 1], xs_, ALU.mult, ALU.add, r=[py.name, "X%d" % t, "gates"], w=["X%d" % t])
                        else:
                            TT(xs_, py.ap, xs_, ALU.add, r=[py.name, "X%d" % t], w=["X%d" % t])
                        yield

            N = len(groups)
            for _ in up(0):
                pass
            for n in range(N):
                ug = up(n + 1) if n + 1 < N else None
                dg = down(n)
                for step in range(8):
                    if ug is not None:
                        next(ug, None)
                    for _ in range(4):
                        next(dg, None)
                if ug is not None:
                    for _ in ug:
                        pass
                for _ in dg:
                    pass
                if n + 2 < N:
                    load_up(n + 2, slot_of(n + 2))
                    load_dn(n + 2, slot_of(n + 2))

        gens = []
        for sq in range(nseq):
            gens.append(phase_load(sq))
            for l in layers:
                gens.append(phase_norm(l, 0))
                gens.append(phase_attn(l, 0))
                gens.append(phase_attn(l, 1))
                gens.append(phase_conf(l))
                gens.append(phase_gmlp(l))
                gens.append(phase_lru(l))
                gens.append(phase_norm(l, 8))
                if l % 2 == 1:
                    gens.append(phase_router(l))
                gens.append(phase_ffn(l, l % 2 == 1))
            gens.append(phase_store(sq))
        if n_phases is not None:
            gens = gens[:n_phases] + [phase_store(0)]
        next(gens[0])
        for i, g in enumerate(gens):
            for _ in g:
                pass
            if i + 1 < len(gens):
                next(gens[i + 1])
            P.fence()
        P.wait_all("sp", stores)
        P.emit(st)
    return nc


def prep_shared(inp):
    f = lambda a: np.ascontiguousarray(np.asarray(a, dtype=np.float32))
    pcol = np.zeros((128, L * NPC), np.float32)

    def col(v, n):
        return np.asarray(v, np.float32).reshape(n, 128).T

    for l in range(L):
        b = l * NPC
        pcol[:, b + 0:b + 8] = col(inp["norm1_g"][l], 8)
        pcol[:, b + 8:b + 16] = col(inp["norm2_g"][l], 8)
        pcol[:, b + 16:b + 24] = col(inp["group_norm_g"][l], 8)
        pcol[:, b + 24:b + 26] = col(inp["conf_dw_b"][l], 2)
        pcol[:, b + 26:b + 28] = col(inp["conf_ln_g"][l], 2)
        pcol[:, b + 28:b + 30] = col(inp["conf_ln_b"][l], 2)
        pcol[:, b + 30:b + 32] = col(inp["lru_conv_b"][l], 2)
        pcol[:, b + 32:b + 34] = col(inp["lru_ba"][l], 2)
        pcol[:, b + 34:b + 36] = col(inp["lru_bx"][l], 2)
        pcol[:, b + 36:b + 38] = col(inp["lru_lambda"][l], 2)
        pcol[:, b + 38] = np.tile(np.asarray(inp["q_norm_g"][l], np.float32), 2)
        pcol[:, b + 39] = np.tile(np.asarray(inp["k_norm_g"][l], np.float32), 2)
        cw = np.asarray(inp["conf_dw_w"][l], np.float32)
        lw = np.asarray(inp["lru_conv_w"][l], np.float32)
        for c in range(2):
            pcol[:, b + 40 + 31 * c:b + 40 + 31 * c + 31] = cw[:, 128 * c:128 * c + 128].T
            pcol[:, b + 102 + 4 * c:b + 102 + 4 * c + 4] = lw[:, 128 * c:128 * c + 128].T
    pbc = np.zeros((L, 1, 1536), np.float32)
    wsT = np.zeros((L, 128, 512), np.float32)
    bsT = np.zeros((L, 128, 256), np.float32)
    for l in range(L):
        pbc[l, 0, 0:256] = inp["gmlp_ln_g"][l]
        pbc[l, 0, 256:512] = inp["gmlp_ln_b"][l]
        pbc[l, 0, 512:1536] = inp["norm2_g"][l]
        wsT[l] = np.asarray(inp["gmlp_ws"][l], np.float32).transpose(2, 0, 1).reshape(128, 512)
        bsT[l] = np.repeat(np.asarray(inp["gmlp_bs"][l], np.float32), 64, axis=0).reshape(2, 128, 128).transpose(1, 0, 2).reshape(128, 256)
    routerF = np.ascontiguousarray(np.asarray(inp["moe_router"][0], np.float32).reshape(8, 128, NE).transpose(1, 0, 2)).reshape(128, 8 * NE)
    return {
        "w_in": f(inp["w_in"]), "w_out": f(inp["w_out"]),
        "ffn_w_gate": f(inp["ffn_w_gate"]), "ffn_w_up": f(inp["ffn_w_up"]), "ffn_w_down": f(inp["ffn_w_down"]),
        "moe_w_gate": f(inp["moe_w_gate"]), "moe_w_up": f(inp["moe_w_up"]), "moe_w_down": f(inp["moe_w_down"]),
        "pcol": pcol, "pbc": pbc, "wsT": wsT, "bsT": bsT, "routerF": routerF,
        "lru_wa": f(inp["lru_wa"]), "lru_wx": f(inp["lru_wx"]),
    }


def kernel(**inputs):
    x = np.ascontiguousarray(np.asarray(inputs["x"], dtype=np.float32))
    B = x.shape[0]
    nseq = B // N_CORES
    shared = prep_shared(inputs)
    nc = build_nc(nseq=nseq)
    in_maps = []
    for c in range(N_CORES):
        m = dict(shared)
        m["x"] = x[c * nseq:(c + 1) * nseq]
        in_maps.append(m)
    res = run_bass_kernel_spmd(nc, in_maps, core_ids=list(range(N_CORES)))
    out = np.concatenate([np.asarray(r["y"], dtype=np.float32) for r in res.results], axis=0)
    return out
```
